# Optimizing a Trainium2 kernel written in Bass

```python
import numpy as np
import jax
import jax.numpy as jnp
from jax import lax

D_MODEL = 2048
BATCH = 4
SEQ = 4096
DEPTH = 4

HEAD_DIM = 128
Q_BLOCK = 128

MLA_HEADS = 8
MLA_Q_RANK = 512
MLA_KV_RANK = 256
MLA_NOPE = 128
MLA_ROPE = 64
MLA_V = 128
ROPE_THETA = 10000.0

NSA_HEADS = 8
NSA_GROUPS = 2
NSA_CMP_LEN = 32
NSA_CMP_STRIDE = 16
NSA_SEL_LEN = 64
NSA_TOPK = 16
NSA_WINDOW = 512
NSA_Q_CHUNK = 32

SB_HEADS = 8

MEM_LEN = 256
MEM_HEADS = 4

N_EXPERTS = 32
TOP_K = 4
D_EXPERT = 512
SWIGLU_LIMIT = 7.0
SWIGLU_ALPHA = 1.702
MOE_BLOCK = 128

N_BRANCH = 3
BRANCH_WIDTH = 1024
DN_ALPHA = (2 * DEPTH) ** 0.25
DN_BETA = (8 * DEPTH) ** -0.25
LN_EPS = 1e-5
RMS_EPS = 1e-6
NEG = -1e30

IN_SIZES = (MLA_Q_RANK, MLA_KV_RANK, MLA_ROPE,
            NSA_HEADS * HEAD_DIM, 3 * 2 * NSA_GROUPS * HEAD_DIM, 3 * NSA_HEADS,
            3 * SB_HEADS * HEAD_DIM, N_BRANCH * D_MODEL)
N_IN = sum(IN_SIZES)
IN_SPLITS = tuple(int(v) for v in np.cumsum(IN_SIZES)[:-1])

kernel_name = "hybrid_mla_nsa_stickbreak_moe_deepnorm"


def layer_norm(x, g, b):
    xf = x.astype(jnp.float32)
    mu = xf.mean(-1, keepdims=True)
    var = jnp.square(xf - mu).mean(-1, keepdims=True)
    y = (xf - mu) * lax.rsqrt(var + LN_EPS) * g.astype(jnp.float32) + b.astype(jnp.float32)
    return y.astype(x.dtype)


def rms_norm(x, g):
    xf = x.astype(jnp.float32)
    y = xf * lax.rsqrt(jnp.mean(xf * xf, -1, keepdims=True) + RMS_EPS) * g.astype(jnp.float32)
    return y.astype(x.dtype)


def masked_softmax(s, mask):
    p = jax.nn.softmax(jnp.where(mask, s, NEG), axis=-1)
    return jnp.where(mask, p, 0.0)


def alibi_slopes(n_heads):
    return np.array([2.0 ** (-8.0 * (h + 1) / n_heads) for h in range(n_heads)], np.float32)


def rope_tables(seq):
    inv = np.asarray(ROPE_THETA ** (-np.arange(0, MLA_ROPE, 2) / MLA_ROPE), np.float32)
    ang = jnp.arange(seq, dtype=jnp.float32)[:, None] * jnp.asarray(inv)[None, :]
    return jnp.cos(ang), jnp.sin(ang)


def apply_rope(x, cos, sin):
    half = x.shape[-1] // 2
    x1, x2 = x[..., :half], x[..., half:]
    c = cos[None, :, None, :].astype(x.dtype)
    s = sin[None, :, None, :].astype(x.dtype)
    return jnp.concatenate([x1 * c - x2 * s, x1 * s + x2 * c], axis=-1)


def causal_softmax_attention(q, k, v, scale):
    B, S, H, _ = q.shape
    nqb = S // Q_BLOCK
    qb = q.reshape(B, nqb, Q_BLOCK, H, q.shape[-1]).swapaxes(0, 1)
    kpos = jnp.arange(S)

    def block(args):
        i, qi = args
        s = jnp.einsum('bqhd,bkhd->bhqk', qi, k, preferred_element_type=jnp.float32) * scale
        tpos = i * Q_BLOCK + jnp.arange(Q_BLOCK)
        p = masked_softmax(s, kpos[None, :] <= tpos[:, None])
        return jnp.einsum('bhqk,bkhd->bqhd', p.astype(v.dtype), v)

    o = lax.map(block, (jnp.arange(nqb), qb))
    return o.swapaxes(0, 1).reshape(B, S, H, v.shape[-1])


def stick_breaking_attention(q, k, v):
    B, S, H, dk = q.shape
    scale = dk ** -0.5
    nqb = S // Q_BLOCK
    qb = q.reshape(B, nqb, Q_BLOCK, H, dk).swapaxes(0, 1)
    kpos = jnp.arange(S)

    def block(args):
        i, qi = args
        z = jnp.einsum('bqhd,bkhd->bhqk', qi, k, preferred_element_type=jnp.float32) * scale
        tpos = i * Q_BLOCK + jnp.arange(Q_BLOCK)
        strict = kpos[None, :] < tpos[:, None]
        log_1m = jnp.where(strict, jax.nn.log_sigmoid(-z), 0.0)
        between = lax.cumsum(log_1m, axis=3, reverse=True) - log_1m
        a = jnp.where(strict, jnp.exp(jax.nn.log_sigmoid(z) + between), 0.0)
        return jnp.einsum('bhqk,bkhd->bqhd', a.astype(v.dtype), v)

    o = lax.map(block, (jnp.arange(nqb), qb))
    return o.swapaxes(0, 1).reshape(B, S, H, dk)


def mla(c_q, c_kv, k_rope, q_norm_g, w_q_up, kv_norm_g, w_kv_up):
    B, S, _ = c_q.shape
    cos, sin = rope_tables(S)
    q = (rms_norm(c_q, q_norm_g) @ w_q_up).reshape(B, S, MLA_HEADS, MLA_NOPE + MLA_ROPE)
    q_rope = apply_rope(q[..., MLA_NOPE:], cos, sin)
    kr = apply_rope(k_rope[:, :, None, :], cos, sin)
    kv = (rms_norm(c_kv, kv_norm_g) @ w_kv_up).reshape(B, S, MLA_HEADS, MLA_NOPE + MLA_V)
    q_full = jnp.concatenate([q[..., :MLA_NOPE], q_rope], axis=-1)
    k_full = jnp.concatenate([kv[..., :MLA_NOPE],
                              jnp.broadcast_to(kr, (B, S, MLA_HEADS, MLA_ROPE))], axis=-1)
    o = causal_softmax_attention(q_full, k_full, kv[..., MLA_NOPE:], (MLA_NOPE + MLA_ROPE) ** -0.5)
    return o.reshape(B, S, MLA_HEADS * MLA_V)


def nsa(q, kv, gate_logits, pe_k, pe_v, w1_k, w1_v, w2_k, w2_v):
    B, S, _ = q.shape
    G, HG, dk = NSA_GROUPS, NSA_HEADS // NSA_GROUPS, HEAD_DIM
    QC, W, L, SL = NSA_Q_CHUNK, NSA_WINDOW, NSA_CMP_LEN, NSA_SEL_LEN
    scale = dk ** -0.5
    dt = q.dtype
    qg = q.reshape(B, S, G, HG, dk)
    kv = kv.reshape(B, S, 3, 2, G, dk)
    gates = jax.nn.sigmoid(gate_logits.reshape(B, S, G, HG, 3))
    slopes = jnp.asarray(alibi_slopes(NSA_HEADS)).reshape(G, HG)

    n_cmp = (S - L) // NSA_CMP_STRIDE + 1
    cmp_idx = np.arange(n_cmp)[:, None] * NSA_CMP_STRIDE + np.arange(L)[None, :]

    def compress(raw, pe, w1, w2):
        blk = raw[:, cmp_idx] + pe[None, None, :, None, :]
        blk = blk.transpose(0, 1, 3, 2, 4).reshape(B, n_cmp, G, L * dk)
        return jax.nn.gelu(blk @ w1) @ w2

    k_cmp = compress(kv[:, :, 0, 0], pe_k, w1_k, w2_k)
    v_cmp = compress(kv[:, :, 0, 1], pe_v, w1_v, w2_v)
    cmp_end = jnp.asarray(cmp_idx[:, -1])
    cmp_mid = jnp.asarray(cmp_idx.mean(1), jnp.float32)

    n_sel = S // SL
    n_top = min(NSA_TOPK, n_sel)
    cs = np.arange(n_cmp) * NSA_CMP_STRIDE
    ss = np.arange(n_sel) * SL
    ov = np.clip(np.minimum(cs[:, None] + L, ss[None, :] + SL) - np.maximum(cs[:, None], ss[None, :]), 0, None) / L
    overlap = jnp.asarray(ov, jnp.float32)
    k_sel = kv[:, :, 1, 0].reshape(B, n_sel, SL, G, dk).transpose(0, 3, 1, 2, 4)
    v_sel = kv[:, :, 1, 1].reshape(B, n_sel, SL, G, dk).transpose(0, 3, 1, 2, 4)
    b_ix = jnp.arange(B)[:, None, None, None]
    g_ix = jnp.arange(G)[None, :, None, None]
    blk_ids = jnp.arange(n_sel)
    tok_in_blk = jnp.arange(SL)

    pad = ((0, 0), (W, 0), (0, 0), (0, 0))
    k_win = jnp.pad(kv[:, :, 2, 0], pad)
    v_win = jnp.pad(kv[:, :, 2, 1], pad)

    def chunk(c):
        t0 = c * QC
        tpos = t0 + jnp.arange(QC)
        qc = lax.dynamic_slice_in_dim(qg, t0, QC, axis=1)

        s_c = jnp.einsum('bqghd,bngd->bghqn', qc, k_cmp, preferred_element_type=jnp.float32) * scale
        s_c = s_c - slopes[None, :, :, None, None] * (tpos[:, None].astype(jnp.float32) - cmp_mid[None, :])
        p_c = masked_softmax(s_c, cmp_end[None, :] <= tpos[:, None])
        o_c = jnp.einsum('bghqn,bngd->bqghd', p_c.astype(dt), v_cmp)

        imp = jnp.einsum('bghqn,nj->bgqj', p_c, overlap)
        cur = tpos // SL
        forced = (blk_ids[None, :] == 0) | (blk_ids[None, :] == cur[:, None]) | (blk_ids[None, :] == cur[:, None] - 1)
        imp = jnp.where(blk_ids[None, :] > cur[:, None], -jnp.inf, jnp.where(forced, jnp.inf, imp))
        sel = lax.top_k(imp, n_top)[1]
        kg = k_sel[b_ix, g_ix, sel]
        vg = v_sel[b_ix, g_ix, sel]
        spos = sel[..., None] * SL + tok_in_blk
        s_s = jnp.einsum('bqghd,bgqkld->bghqkl', qc, kg, preferred_element_type=jnp.float32) * scale
        dist_s = (tpos[None, None, :, None, None] - spos).astype(jnp.float32)[:, :, None]
        s_s = s_s - slopes[None, :, :, None, None, None] * dist_s
        mask_s = (spos <= tpos[None, None, :, None, None])[:, :, None]
        p_s = masked_softmax(s_s.reshape(B, G, HG, QC, n_top * SL), mask_s.reshape(B, G, 1, QC, n_top * SL))
        o_s = jnp.einsum('bghqkl,bgqkld->bqghd', p_s.reshape(B, G, HG, QC, n_top, SL).astype(dt), vg)

        kw = lax.dynamic_slice_in_dim(k_win, t0, QC + W, axis=1)
        vw = lax.dynamic_slice_in_dim(v_win, t0, QC + W, axis=1)
        wpos = t0 - W + jnp.arange(QC + W)
        dw = tpos[:, None] - wpos[None, :]
        s_w = jnp.einsum('bqghd,bkgd->bghqk', qc, kw, preferred_element_type=jnp.float32) * scale
        s_w = s_w - slopes[None, :, :, None, None] * dw.astype(jnp.float32)
        p_w = masked_softmax(s_w, (dw >= 0) & (dw < W) & (wpos[None, :] >= 0))
        o_w = jnp.einsum('bghqk,bkgd->bqghd', p_w.astype(dt), vw)

        gc = lax.dynamic_slice_in_dim(gates, t0, QC, axis=1)
        return gc[..., 0:1] * o_c + gc[..., 1:2] * o_s + gc[..., 2:3] * o_w

    o = lax.map(chunk, jnp.arange(S // QC))
    return o.swapaxes(0, 1).reshape(B, S, NSA_HEADS * dk)


def hybrid_mixer(x, w_in, mla_q_norm, mla_w_q_up, mla_kv_norm, mla_w_kv_up,
                 nsa_pe_k, nsa_pe_v, nsa_w1_k, nsa_w1_v, nsa_w2_k, nsa_w2_v,
                 w_branch, b_merge, w_out):
    B, S, _ = x.shape
    h = x @ w_in
    c_q, c_kv, k_rope, nsa_q, nsa_kv, nsa_gate, sb_qkv, merge = jnp.split(h, IN_SPLITS, axis=-1)
    o_a = mla(c_q, c_kv, k_rope, mla_q_norm, mla_w_q_up, mla_kv_norm, mla_w_kv_up)
    o_b = nsa(nsa_q, nsa_kv, nsa_gate, nsa_pe_k, nsa_pe_v, nsa_w1_k, nsa_w1_v, nsa_w2_k, nsa_w2_v)
    sb = sb_qkv.reshape(B, S, 3, SB_HEADS, HEAD_DIM)
    o_c = stick_breaking_attention(sb[:, :, 0], sb[:, :, 1], sb[:, :, 2]).reshape(B, S, BRANCH_WIDTH)
    g = jax.nn.sigmoid(merge.reshape(B, S, N_BRANCH, D_MODEL) + b_merge)
    y = (g[:, :, 0] * (o_a @ w_branch[0]) + g[:, :, 1] * (o_b @ w_branch[1])
         + g[:, :, 2] * (o_c @ w_branch[2]))
    return y @ w_out


def memory_attention(x, mem, w_q, w_k, w_v, w_o):
    B, S, _ = x.shape
    M = mem.shape[1]
    q = (x @ w_q).reshape(B, S, MEM_HEADS, HEAD_DIM)
    k = (mem @ w_k).reshape(B, M, MEM_HEADS, HEAD_DIM)
    v = (mem @ w_v).reshape(B, M, MEM_HEADS, HEAD_DIM)
    s = jnp.einsum('bshd,bmhd->bhsm', q, k, preferred_element_type=jnp.float32) * HEAD_DIM ** -0.5
    p = jax.nn.softmax(s, axis=-1)
    o = jnp.einsum('bhsm,bmhd->bshd', p.astype(v.dtype), v).reshape(B, S, MEM_HEADS * HEAD_DIM)
    return o @ w_o


def moe(x, w_router, b_router, w_gate, b_gate, w_up, b_up, w_down, b_down):
    B, S, D = x.shape
    T = B * S
    xf = x.reshape(T, D)
    logits = jnp.matmul(xf, w_router, preferred_element_type=jnp.float32) + b_router.astype(jnp.float32)
    top_val, top_idx = lax.top_k(logits, TOP_K)
    top_w = jax.nn.softmax(top_val, axis=-1).astype(x.dtype)
    n_assign = T * TOP_K
    flat_e = top_idx.reshape(-1)
    flat_tok = (jnp.arange(n_assign) // TOP_K).astype(jnp.int32)
    order = jnp.argsort(flat_e)
    e_sorted = flat_e[order]
    counts = jnp.bincount(flat_e, length=N_EXPERTS)
    padded = (counts + MOE_BLOCK - 1) // MOE_BLOCK * MOE_BLOCK
    pad_end = jnp.cumsum(padded)
    pad_start = pad_end - padded
    start = jnp.cumsum(counts) - counts
    dest = pad_start[e_sorted] + jnp.arange(n_assign) - start[e_sorted]
    n_rows = -(-n_assign // MOE_BLOCK) * MOE_BLOCK + N_EXPERTS * MOE_BLOCK
    n_blocks = n_rows // MOE_BLOCK
    row_tok = jnp.full((n_rows,), T, jnp.int32).at[dest].set(flat_tok[order])
    row_w = jnp.zeros((n_rows,), x.dtype).at[dest].set(top_w.reshape(-1)[order])
    blk_e = jnp.minimum(jnp.searchsorted(pad_end, jnp.arange(n_blocks) * MOE_BLOCK, side='right'), N_EXPERTS - 1)
    x_pad = jnp.concatenate([xf, jnp.zeros((1, D), x.dtype)], axis=0)

    def expert_block(args):
        tok, e = args
        xb = x_pad[tok]
        g = jnp.minimum(xb @ w_gate[e] + b_gate[e], SWIGLU_LIMIT)
        u = jnp.clip(xb @ w_up[e] + b_up[e], -SWIGLU_LIMIT, SWIGLU_LIMIT)
        hdn = (u + 1.0) * (g * jax.nn.sigmoid(SWIGLU_ALPHA * g))
        return hdn @ w_down[e] + b_down[e]

    y_rows = lax.map(expert_block, (row_tok.reshape(n_blocks, MOE_BLOCK), blk_e))
    y = jnp.zeros((T + 1, D), x.dtype).at[row_tok].add(y_rows.reshape(n_rows, D) * row_w[:, None])
    return y[:T].reshape(B, S, D)


def setup_inputs(seed: int = 0) -> dict:
    key = jax.random.key(seed)
    ks = iter(jax.random.split(key, 48))
    L, D, F, E = DEPTH, D_MODEL, D_EXPERT, N_EXPERTS

    def nrm(shape, scale):
        return jax.random.normal(next(ks), shape, jnp.float32) * scale

    def gain(shape):
        return 1.0 + nrm(shape, 0.02)

    return {
        "x": nrm((BATCH, SEQ, D), 1.0),
        "mem": nrm((BATCH, MEM_LEN, D), 1.0),
        "w_in": nrm((L, D, N_IN), D ** -0.5),
        "mla_q_norm": gain((L, MLA_Q_RANK)),
        "mla_w_q_up": nrm((L, MLA_Q_RANK, MLA_HEADS * (MLA_NOPE + MLA_ROPE)), MLA_Q_RANK ** -0.5),
        "mla_kv_norm": gain((L, MLA_KV_RANK)),
        "mla_w_kv_up": nrm((L, MLA_KV_RANK, MLA_HEADS * (MLA_NOPE + MLA_V)), MLA_KV_RANK ** -0.5),
        "nsa_pe_k": nrm((L, NSA_CMP_LEN, HEAD_DIM), 0.1),
        "nsa_pe_v": nrm((L, NSA_CMP_LEN, HEAD_DIM), 0.1),
        "nsa_w1_k": nrm((L, NSA_CMP_LEN * HEAD_DIM, HEAD_DIM), (NSA_CMP_LEN * HEAD_DIM) ** -0.5),
        "nsa_w1_v": nrm((L, NSA_CMP_LEN * HEAD_DIM, HEAD_DIM), (NSA_CMP_LEN * HEAD_DIM) ** -0.5),
        "nsa_w2_k": nrm((L, HEAD_DIM, HEAD_DIM), HEAD_DIM ** -0.5),
        "nsa_w2_v": nrm((L, HEAD_DIM, HEAD_DIM), HEAD_DIM ** -0.5),
        "w_branch": nrm((L, N_BRANCH, BRANCH_WIDTH, D), DN_BETA * BRANCH_WIDTH ** -0.5),
        "b_merge": nrm((L, N_BRANCH, D), 0.02),
        "w_out": nrm((L, D, D), DN_BETA * D ** -0.5),
        "ln_mix_g": gain((L, D)),
        "ln_mix_b": nrm((L, D), 0.02),
        "mem_w_q": nrm((L, D, MEM_HEADS * HEAD_DIM), D ** -0.5),
        "mem_w_k": nrm((L, D, MEM_HEADS * HEAD_DIM), D ** -0.5),
        "mem_w_v": nrm((L, D, MEM_HEADS * HEAD_DIM), DN_BETA * D ** -0.5),
        "mem_w_o": nrm((L, MEM_HEADS * HEAD_DIM, D), DN_BETA * (MEM_HEADS * HEAD_DIM) ** -0.5),
        "ln_mem_g": gain((L, D)),
        "ln_mem_b": nrm((L, D), 0.02),
        "moe_w_router": nrm((L, D, E), D ** -0.5),
        "moe_b_router": nrm((L, E), 0.01),
        "moe_w_gate": nrm((L, E, D, F), D ** -0.5),
        "moe_b_gate": nrm((L, E, F), 0.02),
        "moe_w_up": nrm((L, E, D, F), D ** -0.5),
        "moe_b_up": nrm((L, E, F), 0.02),
        "moe_w_down": nrm((L, E, F, D), DN_BETA * F ** -0.5),
        "moe_b_down": nrm((L, E, D), 0.02),
        "ln_moe_g": gain((L, D)),
        "ln_moe_b": nrm((L, D), 0.02),
    }


def reference(x, mem, w_in, mla_q_norm, mla_w_q_up, mla_kv_norm, mla_w_kv_up,
              nsa_pe_k, nsa_pe_v, nsa_w1_k, nsa_w1_v, nsa_w2_k, nsa_w2_v,
              w_branch, b_merge, w_out, ln_mix_g, ln_mix_b,
              mem_w_q, mem_w_k, mem_w_v, mem_w_o, ln_mem_g, ln_mem_b,
              moe_w_router, moe_b_router, moe_w_gate, moe_b_gate, moe_w_up, moe_b_up,
              moe_w_down, moe_b_down, ln_moe_g, ln_moe_b):
    for l in range(DEPTH):
        h = hybrid_mixer(x, w_in[l], mla_q_norm[l], mla_w_q_up[l], mla_kv_norm[l], mla_w_kv_up[l],
                         nsa_pe_k[l], nsa_pe_v[l], nsa_w1_k[l], nsa_w1_v[l], nsa_w2_k[l], nsa_w2_v[l],
                         w_branch[l], b_merge[l], w_out[l])
        x = layer_norm(DN_ALPHA * x + h, ln_mix_g[l], ln_mix_b[l])
        h = memory_attention(x, mem, mem_w_q[l], mem_w_k[l], mem_w_v[l], mem_w_o[l])
        x = layer_norm(DN_ALPHA * x + h, ln_mem_g[l], ln_mem_b[l])
        h = moe(x, moe_w_router[l], moe_b_router[l], moe_w_gate[l], moe_b_gate[l],
                moe_w_up[l], moe_b_up[l], moe_w_down[l], moe_b_down[l])
        x = layer_norm(DN_ALPHA * x + h, ln_moe_g[l], ln_moe_b[l])
    return x
```

```python
FUSED = True
import numpy as np
import concourse.bass as bass
import concourse.mybir as mybir

F32 = mybir.dt.float32
BF16 = mybir.dt.bfloat16
AF = mybir.ActivationFunctionType
ALU = mybir.AluOpType
AX = mybir.AxisListType

ENGS = ("pe", "act", "dve", "pool", "sp")
N_DMA_SEMS = 24
N_SW_SEMS = 8


class Buf:
    def __init__(self, h, name):
        self.h = h
        self.name = name
        self.lw = None
        self.rd = []

    def __getitem__(self, idx):
        return self.h[idx]

    def ap(self):
        return self.h.ap() if hasattr(self.h, "ap") else self.h[:]


class KB:
    def __init__(self):
        self.nc = bass.Bass("TRN2", target_bir_lowering=False)
        self.ops = {e: [] for e in ENGS}
        self.cnt = {e: 0 for e in ENGS}
        self.known = {e: {} for e in ENGS}
        self.dma_cnt = [0] * N_DMA_SEMS
        self.dma_rr = 0
        self.sw_rr = 0
        self.n_sb = 0
        self.sb_off = 16640

    def sb(self, name, shape, dtype=F32):
        nbytes = int(np.prod(shape[1:])) * (2 if dtype == BF16 else 4)
        off = (self.sb_off + 63) // 64 * 64
        assert off + nbytes <= 229000, ("SBUF overflow", name, off, nbytes)
        self.sb_off = off + nbytes
        self.n_sb += 1
        h = self.nc.alloc_sbuf_tensor_at("%s_%d" % (name, self.n_sb), list(shape), dtype, offset=off)
        return Buf(h, name)

    def mark(self):
        return self.sb_off

    def release(self, m):
        self.barrier()
        self.sb_off = m

    def barrier(self):
        for e in ENGS:
            waits = []
            for e2 in ENGS:
                key, val = ("e", e2), self.cnt[e2]
                if e2 != e and val > 0 and self.known[e].get(key, 0) < val:
                    waits.append((key, val)); self.known[e][key] = val
            for j in range(N_DMA_SEMS):
                key, val = ("d", j), self.dma_cnt[j]
                if val > 0 and self.known[e].get(key, 0) < val:
                    waits.append((key, val)); self.known[e][key] = val
            if waits:
                self.ops[e].append((waits, None, None, 0))

    def ps(self, name, shape, dtype=F32):
        return Buf(self.nc.alloc_psum_tensor(name, list(shape), dtype), name)

    def dram(self, name, shape, dtype=F32, kind="Internal"):
        return Buf(self.nc.dram_tensor(name, list(shape), dtype, kind=kind), name)

    def _deps(self, eng, reads, writes):
        deps = set()
        for b in reads:
            if b.lw is not None:
                deps.add(b.lw)
        for b in writes:
            if b.lw is not None:
                deps.add(b.lw)
            deps.update(b.rd)
        waits = {}
        for d in deps:
            key, val = d
            if key == ("e", eng) and eng == "pe":
                continue
            if self.known[eng].get(key, 0) >= val:
                continue
            waits[key] = max(waits.get(key, 0), val)
        for k, v in waits.items():
            self.known[eng][k] = v
        return list(waits.items())

    def _mark(self, tok, reads, writes):
        for b in reads:
            b.rd.append(tok)
        for b in writes:
            b.lw = tok
            b.rd = []

    def op(self, eng, emit, reads=(), writes=()):
        waits = self._deps(eng, reads, writes)
        self.cnt[eng] += 1
        tok = (("e", eng), self.cnt[eng])
        self.ops[eng].append((waits, emit, ("e", eng), 1))
        self._mark(tok, reads, writes)
        return tok

    def dma(self, eng, out_ap, in_ap, reads=(), writes=(), **kw):
        waits = self._deps(eng, reads, writes)
        if eng == "pool":
            j = N_DMA_SEMS - N_SW_SEMS + self.sw_rr
            self.sw_rr = (self.sw_rr + 1) % N_SW_SEMS
            key, val = ("d", j), self.dma_cnt[j]
            if val > 0 and self.known[eng].get(key, 0) < val:
                waits = [w for w in waits if w[0] != key] + [(key, val)]
                self.known[eng][key] = val
        else:
            j = self.dma_rr
            self.dma_rr = (self.dma_rr + 1) % (N_DMA_SEMS - N_SW_SEMS)
        self.dma_cnt[j] += 16
        tok = (("d", j), self.dma_cnt[j])
        self.ops[eng].append((waits, lambda e: e.dma_start(out=out_ap, in_=in_ap, **kw), ("d", j), 16))
        self._mark(tok, reads, writes)
        return tok

    def cc(self, emit, reads=(), writes=()):
        eng = "pool"
        waits = self._deps(eng, reads, writes)
        self.cnt[eng] += 1
        tok = (("e", eng), self.cnt[eng])
        self.ops[eng].append((waits, emit, ("e", eng), None))
        self._mark(tok, reads, writes)
        return tok

    def wait_all(self, eng, bufs):
        waits = self._deps(eng, bufs, bufs)
        self.ops[eng].append((waits, None, None, 0))

    def finish(self):
        nc = self.nc
        esem = {e: nc.alloc_semaphore("es_" + e) for e in ENGS}
        dsem = [nc.alloc_semaphore("ds_%d" % j) for j in range(N_DMA_SEMS)]

        def sem_of(key):
            return esem[key[1]] if key[0] == "e" else dsem[key[1]]

        def run(engine, lst):
            for waits, emit, skey, inc in lst:
                for key, val in waits:
                    engine.wait_ge(sem_of(key), val)
                if emit is None:
                    continue
                ins = emit(engine)
                if inc is None:
                    ins.then_inc(sem_of(skey))
                else:
                    ins.then_inc(sem_of(skey), inc)

        with nc.Block() as block:
            @block.tensor
            def _(e):
                run(e, self.ops["pe"])

            @block.scalar
            def _(e):
                run(e, self.ops["act"])

            @block.vector
            def _(e):
                run(e, self.ops["dve"])

            @block.gpsimd
            def _(e):
                run(e, self.ops["pool"])

            @block.sync
            def _(e):
                run(e, self.ops["sp"])
        return nc


D = 2048
S = 4096
T = 2048
NQC = 4
OWN = ((0, 3, 4, 7), (1, 2, 5, 6))
OWNER = (0, 1, 1, 0, 0, 1, 1, 0)
LCH = (0, 0, 1, 1, 2, 2, 3, 3)
JMAX = (1, 3, 5, 7)
N_IN = 12632
OFF_CQ, OFF_CKV, OFF_KR, OFF_NQ, OFF_NKV, OFF_NG, OFF_SB, OFF_MG = 0, 512, 768, 832, 1856, 3392, 3416, 6488
NEGM = -30000.0
DN_ALPHA = 8 ** 0.25
SLOPES = [2.0 ** (-(h + 1)) for h in range(8)]

WSPEC = {
    "w_in": (2048, N_IN), "mla_w_q_up": (512, 1536), "mla_w_kv_up": (256, 2048),
    "nsa_w1_k": (4096, 128), "nsa_w1_v": (4096, 128), "nsa_w2_k": (128, 128), "nsa_w2_v": (128, 128),
    "w_branch": (3072, 2048), "w_out": (2048, 2048),
    "mem_w_q": (2048, 512), "mem_w_k": (2048, 512), "mem_w_v": (2048, 512), "mem_w_o": (512, 2048),
    "moe_w_router": (2048, 32), "moe_w_gate": (65536, 512), "moe_w_up": (65536, 512), "moe_w_down": (16384, 2048),
}
VSPEC = {
    "mla_q_norm": 512, "mla_kv_norm": 256, "nsa_pe_k": 4096, "nsa_pe_v": 4096, "b_merge": 6144,
    "ln_mix_g": 2048, "ln_mix_b": 2048, "ln_mem_g": 2048, "ln_mem_b": 2048, "ln_moe_g": 2048, "ln_moe_b": 2048,
    "moe_b_router": 32, "moe_b_gate": 16384, "moe_b_up": 16384, "moe_b_down": 65536,
}


class MK:
    def __init__(self, L, stages="all", debug=()):
        self.L = L
        self.kb = kb = KB()
        self.debug = debug
        self.rr = 0
        ein = lambda n, s, dt=F32: kb.dram(n, s, dt, kind="ExternalInput")
        self.x_in = ein("x_own", [T, D])
        self.mem_in = ein("mem_b", [256, D])
        self.w_in = {n: ein("ws_" + n, [L * r // 8, c]) for n, (r, c) in WSPEC.items()}
        self.v_in = {n: ein("vs_" + n, [L, ln]) for n, ln in VSPEC.items()}
        self.t_ident = ein("t_ident", [128, 128])
        self.t_tri = ein("t_tri", [128, 256])
        self.t_rope_own = ein("t_rope_own", [T, 64])
        self.t_rope_all = ein("t_rope_all", [S, 64])
        self.t_maskI = ein("t_maskI", [NQC * 8 * 128, 512])
        self.t_maskS = ein("t_maskS", [NQC * 8 * 128, 512])
        self.t_maskW = ein("t_maskW", [NQC * 12 * 128, 512])
        self.t_maskC = ein("t_maskC", [NQC * 2 * 128, 512])
        self.t_alk = ein("t_alk", [4, 8 * 128])
        self.t_alq = ein("t_alq", [4, 8 * 512])
        self.t_alck = ein("t_alck", [4, 8 * 128])
        self.t_bcol = ein("t_bcol", [128, NQC * 32 * 8])
        self.t_bcolc = ein("t_bcolc", [128, NQC * 2 * 8])
        self.t_fb = ein("t_fb", [T, 64])
        self.t_ov = ein("t_ov", [256, 64])
        self.t_et = ein("t_et", [64, S])
        self.out = kb.dram("y_out", [T, D], F32, kind="ExternalOutput")
        self.dbg = {}
        self.P = [kb.ps("pb%d" % i, [128, 512], F32) for i in range(3)]
        self.PO = [kb.ps("po%d" % c, [128, 512], F32) for c in range(4)]
        self.PT = kb.ps("ptr", [128, 1024], BF16)
        self.pi = 0

    def tmp(self, name, shape, dtype=F32):
        if not hasattr(self, "_tmp"):
            self._tmp = {}
        if name not in self._tmp:
            self._tmp[name] = self.kb.sb(name, shape, dtype)
        return self._tmp[name]

    def end_stage(self, m):
        self.kb.release(m)
        self._tmp = {}

    def nextP(self):
        p = self.P[self.pi % 3]
        self.pi += 1
        return p

    def evac(self, out_ap, in_ap, reads, writes, scale=None):
        self.rr += 1
        if self.rr % 2 == 0:
            if scale is None:
                self.kb.op("act", lambda e: e.copy(out=out_ap, in_=in_ap), reads, writes)
            else:
                self.kb.op("act", lambda e: e.mul(out=out_ap, in_=in_ap, mul=scale), reads, writes)
        else:
            if scale is None:
                self.kb.op("dve", lambda e: e.tensor_copy(out=out_ap, in_=in_ap), reads, writes)
            else:
                self.kb.op("dve", lambda e: e.tensor_scalar_mul(out=out_ap, in0=in_ap, scalar1=scale), reads, writes)

    def dbg_out(self, name, shape, dtype, src):
        if name in self.debug:
            o = self.kb.dram("dbg_" + name, shape, dtype, kind="ExternalOutput")
            self.kb.dma("sp", o.ap(), src.ap(), reads=[src], writes=[o])
            self.dbg[name] = o

    def consts(self):
        kb = self.kb
        c = self.c = {}

        def load_bf(name, src, shape, rows=None):
            m = kb.mark()
            tmp = kb.sb("ctmp", shape, F32)
            kb.dma("sp", tmp[:], src.ap() if rows is None else src[rows[0]:rows[1], :], reads=[src], writes=[tmp])
            dst = kb.sb(name, shape, BF16)
            kb.op("dve", lambda e: e.tensor_copy(out=dst[:], in_=tmp[:]), [tmp], [dst])
            return dst, m

        c["ident"] = kb.sb("ident", [128, 128], BF16)
        c["tri"] = kb.sb("tri", [128, 256], BF16)
        c["alk"] = kb.sb("alk", [4, 1024], BF16)
        c["alq"] = kb.sb("alq", [4, 4096], BF16)
        c["alck"] = kb.sb("alck", [4, 1024], BF16)
        c["bcolc"] = kb.sb("bcolc", [128, NQC * 2 * 8], F32)
        kb.dma("sp", c["bcolc"][:], self.t_bcolc.ap(), reads=[self.t_bcolc], writes=[c["bcolc"]])
        c["eps"] = kb.sb("epsc", [128, 4], F32)
        kb.op("dve", lambda e: e.memset(c["eps"][:, 0:1], 1e-6), [], [c["eps"]])
        kb.op("dve", lambda e: e.memset(c["eps"][:, 1:2], 1e-5), [c["eps"]], [c["eps"]])
        kb.op("dve", lambda e: e.memset(c["eps"][:, 2:3], 1.0), [c["eps"]], [c["eps"]])
        kb.op("dve", lambda e: e.memset(c["eps"][:, 3:4], 0.0), [c["eps"]], [c["eps"]])
        mC_ = kb.mark()
        for nm, src, shp in (("ident", self.t_ident, [128, 128]), ("tri", self.t_tri, [128, 256]), ("alk", self.t_alk, [4, 1024]),
                             ("alq", self.t_alq, [4, 4096]), ("alck", self.t_alck, [4, 1024])):
            f = kb.sb(nm + "_f", shp, F32)
            kb.dma("sp", f[:], src.ap(), reads=[src], writes=[f])
            kb.op("dve", (lambda dst, f_: lambda e: e.tensor_copy(out=dst[:], in_=f_[:]))(c[nm], f), [f], [c[nm]])
        kb.release(mC_)
        self.d_mask = {}
        for nm, src, n in (("I", self.t_maskI, NQC * 8), ("S", self.t_maskS, NQC * 8), ("W", self.t_maskW, NQC * 12),
                           ("C", self.t_maskC, NQC * 2)):
            d = kb.dram("maskb_" + nm, [n * 128, 512], BF16)
            kb.dma("pool", d.ap(), src.ap(), reads=[src], writes=[d])
            self.d_mask[nm] = d
        self.d_et = kb.dram("etb", [64, S], BF16)
        kb.dma("pool", self.d_et.ap(), self.t_et.ap(), reads=[self.t_et], writes=[self.d_et])
        self.d_ov = kb.dram("ovb", [256, 64], BF16)
        kb.dma("pool", self.d_ov.ap(), self.t_ov.ap(), reads=[self.t_ov], writes=[self.d_ov])

    def gather_weights(self):
        kb = self.kb
        self.W = {}
        for l in range(self.L):
            for n, (r, cdim) in WSPEC.items():
                rs = r // 8
                shard = kb.dram("wsh_%s_%d" % (n, l), [rs, cdim], BF16)
                step = max(1, (1 << 19) // cdim)
                for r0 in range(0, rs, step):
                    r1 = min(rs, r0 + step)
                    kb.dma("pool", shard[r0:r1, :], self.w_in[n][l * rs + r0:l * rs + r1, :],
                           reads=[self.w_in[n]], writes=[shard])
                full = kb.dram("wfull_%s_%d" % (n, l), [r, cdim], BF16)
                kb.cc(lambda e, a=shard, b=full: e.collective_compute(
                    "AllGather", ALU.bypass, replica_groups=[list(range(8))],
                    ins=[a.ap().opt()], outs=[b.ap().opt()]), reads=[shard], writes=[full])
                self.W[(n, l)] = full

    def make_xT(self, src, xT, l, tag):
        kb = self.kb
        m = kb.mark()
        xf = [kb.sb("xf%d" % i, [128, D], F32) for i in range(2)]
        xb = [kb.sb("xb%d" % i, [128, D], BF16) for i in range(2)]
        for tt in range(16):
            f, b = xf[tt % 2], xb[tt % 2]
            kb.dma("sp", f[:], src[tt * 128:(tt + 1) * 128, :], reads=[src], writes=[f])
            kb.op("dve", lambda e, f=f, b=b: e.tensor_copy(out=b[:], in_=f[:]), [f], [b])
            for g in range(2):
                for k in range(8):
                    kc = g * 8 + k
                    kb.op("pe", lambda e, b=b, kc=kc, k=k: e.transpose(
                        out=self.PT[:, k * 128:(k + 1) * 128], in_=b[:, kc * 128:(kc + 1) * 128],
                        identity=self.c["ident"][:]), [b, self.c["ident"]], [self.PT])
                self.evac(xT[:, g * 8:(g + 1) * 8, tt * 128:(tt + 1) * 128],
                          self.PT[:, :].rearrange("p (k t) -> p k t", k=8), [self.PT], [xT])
        kb.release(m)

    def exchange_xT(self, xT, l):
        kb = self.kb
        fulls = []
        for q in range(8):
            bounce = kb.dram("xtb_%d_%d" % (l, q), [256, T], BF16)
            kb.dma("sp", bounce.ap().rearrange("(kc p) t -> p kc t", p=128), xT[:, 2 * q:2 * q + 2, :], reads=[xT], writes=[bounce])
            full = kb.dram("xtg_%d_%d" % (l, q), [512, T], BF16)
            kb.cc(lambda e, a=bounce, f=full: e.collective_compute(
                "AllGather", ALU.bypass, replica_groups=[[0, 1], [2, 3], [4, 5], [6, 7]],
                ins=[a.ap().opt()], outs=[f.ap().opt()]), reads=[bounce], writes=[full])
            fulls.append(full)
        return fulls

    def load_w(self, name, wfull, c0, ncols, kc_n):
        kb = self.kb
        wt = kb.sb(name, [128, kc_n, ncols], BF16)
        kb.dma("sp", wt[:], wfull.ap().rearrange("(kc p) n -> p kc n", p=128)[:, :, c0:c0 + ncols],
               reads=[wfull], writes=[wt])
        return wt

    def bcast_vec(self, name, vsrc, l, off, n):
        kb = self.kb
        t = kb.sb(name, [128, n], F32)
        kb.dma("sp", t[:], vsrc[l:l + 1, off:off + n].broadcast(0, 128) if hasattr(vsrc[l:l + 1, off:off + n], "broadcast")
               else vsrc[l:l + 1, off:off + n], reads=[vsrc], writes=[t])
        return t


def rmsnorm_rows(mk, src, n, gb, out_bf, eps_col):
    kb = mk.kb
    junk = mk.tmp("rn_junk%d" % n, [128, n], F32)
    ss = mk.tmp("rn_ss", [128, 2], F32)
    kb.op("dve", lambda e: e.memset(ss[:], 0.0), [], [ss])
    kb.op("act", lambda e: e.activation(out=junk[:], in_=src[:, 0:n], func=AF.Square, accum_out=ss[:, 0:1]),
          [src, ss], [junk, ss])
    kb.op("act", lambda e: e.activation(out=ss[:, 1:2], in_=ss[:, 0:1], func=AF.Sqrt, scale=1.0 / n,
                                        bias=mk.c["eps"][:, eps_col:eps_col + 1]), [ss, mk.c["eps"]], [ss])
    kb.op("dve", lambda e: e.reciprocal(out=ss[:, 0:1], in_=ss[:, 1:2]), [ss], [ss])
    kb.op("dve", lambda e: e.scalar_tensor_tensor(out=out_bf[:, 0:n], in0=src[:, 0:n], scalar=ss[:, 0:1],
                                                  in1=gb[:, 0:n], op0=ALU.mult, op1=ALU.mult), [src, ss, gb], [out_bf])


def rope_rows(mk, x1, x2, cs, o1, o2, reads, writes, scale=None):
    kb = mk.kb
    t = mk.tmp("rp_t", [128, 4, 32], F32)
    c, s = cs[:, 0:32], cs[:, 32:64]
    kb.op("dve", lambda e: e.tensor_tensor(out=t[:, 0, :], in0=x1, in1=c, op=ALU.mult), reads + [cs], [t])
    kb.op("dve", lambda e: e.tensor_tensor(out=t[:, 1, :], in0=x2, in1=s, op=ALU.mult), reads + [cs, t], [t])
    kb.op("dve", lambda e: e.tensor_tensor(out=t[:, 2, :], in0=x1, in1=s, op=ALU.mult), reads + [cs, t], [t])
    kb.op("dve", lambda e: e.tensor_tensor(out=t[:, 3, :], in0=x2, in1=c, op=ALU.mult), reads + [cs, t], [t])
    if scale is not None:
        kb.op("dve", lambda e: e.tensor_scalar_mul(out=t[:, 0, :], in0=t[:, 0, :], scalar1=scale), [t], [t])
        kb.op("dve", lambda e: e.tensor_scalar_mul(out=t[:, 2, :], in0=t[:, 2, :], scalar1=scale), [t], [t])
        kb.op("dve", lambda e: e.scalar_tensor_tensor(out=o1, in0=t[:, 1, :], scalar=-scale, in1=t[:, 0, :],
                                                      op0=ALU.mult, op1=ALU.add), [t], writes)
        kb.op("dve", lambda e: e.scalar_tensor_tensor(out=o2, in0=t[:, 3, :], scalar=scale, in1=t[:, 2, :],
                                                      op0=ALU.mult, op1=ALU.add), [t], writes)
    else:
        kb.op("dve", lambda e: e.tensor_tensor(out=o1, in0=t[:, 0, :], in1=t[:, 1, :], op=ALU.subtract), [t], writes)
        kb.op("dve", lambda e: e.tensor_tensor(out=o2, in0=t[:, 2, :], in1=t[:, 3, :], op=ALU.add), [t], writes)
    return t


def stage_A(mk, l, xtg):
    kb = mk.kb
    W = mk.W[("w_in", l)]
    m0 = kb.mark()
    KT = mk.KT = {}
    for nm in ["n0", "n1", "n2", "n3", "n4", "n5", "n8", "n9"] + ["sbk%d" % h for h in range(8)] + \
              ["mkn%d" % h for h in range(8)]:
        KT[nm] = kb.dram("KT_%s_%d" % (nm, l), [128, S], BF16)
    KT["mkr"] = kb.dram("KT_mkr_%d" % l, [64, S], BF16)
    VT = mk.VT = {"selv": kb.dram("V_selv_%d" % l, [S, 256], BF16), "winv": kb.dram("V_winv_%d" % l, [S, 256], BF16),
                  "sbv": kb.dram("V_sbv_%d" % l, [S, 1024], BF16), "mv": kb.dram("V_mv_%d" % l, [S, 1024], BF16)}
    fm_groups = [(OFF_NKV, ["n0", "n1", "n2", "n3"]), (OFF_NKV + 512, ["n4", "n5"]), (OFF_NKV + 1024, ["n8", "n9"]),
                 (OFF_SB + 1024, ["sbk0", "sbk1", "sbk2", "sbk3"]), (OFF_SB + 1536, ["sbk4", "sbk5", "sbk6", "sbk7"])]
    tm_groups = [(OFF_CKV, 320, "ckv", 0), (OFF_NKV + 768, 256, "selv", 0), (OFF_NKV + 1280, 256, "winv", 0),
                 (OFF_SB + 2048, 512, "sbv", 0), (OFF_SB + 2560, 512, "sbv", 512)]
    Wkv = mk.W[("mla_w_kv_up", l)]
    wkv_k = kb.sb("wkv_k", [128, 2, 8, 128], BF16)
    wkv_v = kb.sb("wkv_v", [128, 2, 8, 128], BF16)
    wv = Wkv.ap().rearrange("(kc p) (h two d) -> p kc h two d", p=128, two=2, d=128)
    for kc in range(2):
        kb.dma("sp", wkv_k[:, kc], wv[:, kc, :, 0, :], reads=[Wkv], writes=[wkv_k])
        kb.dma("sp", wkv_v[:, kc], wv[:, kc, :, 1, :], reads=[Wkv], writes=[wkv_v])
    gkv = kb.sb("gkv", [128, 256], F32)
    kb.dma("sp", gkv[:], mk.v_in["mla_kv_norm"][l:l + 1, :].partition_broadcast(128), reads=[mk.v_in["mla_kv_norm"]], writes=[gkv])
    XK = [kb.sb("XK%d" % i, [128, 16, 512], BF16) for i in range(2)]
    wts = [kb.sb("wA%d" % i, [128, 16, 512], BF16) for i in range(2)]
    stg = [kb.sb("stgA%d" % i, [128, 512], BF16) for i in range(3)]
    ckvT = kb.sb("ckvT", [128, 2, 512], BF16)
    wi = 0
    si = 0
    wsrc = W.ap().rearrange("(kc p) n -> p kc n", p=128)
    for jk in range(8):
        xk = XK[jk % 2]
        for q in range(8):
            xs = xtg[q].ap().rearrange("(o kc p) t -> o p kc t", o=2, p=128)
            kb.dma("sp", xk[:, 2 * q:2 * q + 2, :], xs[OWNER[jk]][:, :, LCH[jk] * 512:(LCH[jk] + 1) * 512],
                   reads=[xtg[q]], writes=[xk])
        for c0, names in fm_groups:
            wt = wts[wi % 2]; wi += 1
            gw = 128 * len(names)
            kb.dma("sp", wt[:, :, 0:gw], wsrc[:, :, c0:c0 + gw], reads=[W], writes=[wt])
            for sc, nm in enumerate(names):
                ps = mk.nextP()
                for kc in range(16):
                    kb.op("pe", lambda e, ps=ps, wt=wt, xk=xk, kc=kc, sc=sc: e.matmul(
                        ps[:], lhsT=wt[:, kc, sc * 128:(sc + 1) * 128], rhs=xk[:, kc, :], start=(kc == 0), stop=(kc == 15)),
                        [wt, xk], [ps])
                st = stg[si % 3]; si += 1
                mk.evac(st[:], ps[:], [ps], [st])
                kb.dma("sp", KT[nm][:, jk * 512:(jk + 1) * 512], st[:], reads=[st], writes=[KT[nm]])
        for c0, gw, dest, dc0 in tm_groups:
            wt = wts[wi % 2]; wi += 1
            kb.dma("sp", wt[:, :, 0:gw], wsrc[:, :, c0:c0 + gw], reads=[W], writes=[wt])
            for tt in range(4):
                ps = mk.nextP()
                for kc in range(16):
                    kb.op("pe", lambda e, ps=ps, wt=wt, xk=xk, kc=kc, tt=tt, gw=gw: e.matmul(
                        ps[:, 0:gw], lhsT=xk[:, kc, tt * 128:(tt + 1) * 128], rhs=wt[:, kc, 0:gw], start=(kc == 0), stop=(kc == 15)),
                        [wt, xk], [ps])
                r0 = jk * 512 + tt * 128
                if dest != "ckv":
                    st = stg[si % 3]; si += 1
                    mk.evac(st[:, 0:gw], ps[:, 0:gw], [ps], [st])
                    kb.dma("sp", VT[dest][r0:r0 + 128, dc0:dc0 + gw], st[:, 0:gw], reads=[st], writes=[VT[dest]])
                else:
                    ckv = mk.tmp("ckv_f", [128, 320], F32)
                    mk.evac(ckv[:], ps[:, 0:320], [ps], [ckv])
                    ckvn = mk.tmp("ckvn", [128, 256], BF16)
                    rmsnorm_rows(mk, ckv, 256, gkv, ckvn, 0)
                    for k2 in range(2):
                        kb.op("pe", lambda e, k2=k2, ckvn=ckvn: e.transpose(
                            out=mk.PT[:, k2 * 128:(k2 + 1) * 128], in_=ckvn[:, k2 * 128:(k2 + 1) * 128],
                            identity=mk.c["ident"][:]), [ckvn, mk.c["ident"]], [mk.PT])
                    mk.evac(ckvT[:, :, tt * 128:(tt + 1) * 128], mk.PT[:, 0:256].rearrange("p (k t) -> p k t", k=2),
                            [mk.PT], [ckvT])
                    cs = mk.tmp("csA", [128, 64], F32)
                    kb.dma("sp", cs[:], mk.t_rope_all[r0:r0 + 128, :], reads=[mk.t_rope_all], writes=[cs])
                    kr = mk.tmp("krA", [128, 64], BF16)
                    t = rope_rows(mk, ckv[:, 256:288], ckv[:, 288:320], cs, kr[:, 0:32], kr[:, 32:64], [ckv], [kr])
                    kb.op("pe", lambda e, kr=kr: e.transpose(out=mk.PT[0:64, 512:640], in_=kr[:, 0:64],
                                                             identity=mk.c["ident"][:]), [kr, mk.c["ident"]], [mk.PT])
                    st = stg[si % 3]; si += 1
                    mk.evac(st[0:64, 0:128], mk.PT[0:64, 512:640], [mk.PT], [st])
                    kb.dma("sp", KT["mkr"][:, r0:r0 + 128], st[0:64, 0:128], reads=[st], writes=[KT["mkr"]])
        for h in range(8):
            ps = mk.nextP()
            for kc in range(2):
                kb.op("pe", lambda e, ps=ps, kc=kc, h=h: e.matmul(ps[:], lhsT=wkv_k[:, kc, h, :], rhs=ckvT[:, kc, :],
                                                                  start=(kc == 0), stop=(kc == 1)), [wkv_k, ckvT], [ps])
            st = stg[si % 3]; si += 1
            mk.evac(st[:], ps[:], [ps], [st])
            kb.dma("sp", KT["mkn%d" % h][:, jk * 512:(jk + 1) * 512], st[:], reads=[st], writes=[KT["mkn%d" % h]])
        for tt in range(4):
            for hv in range(2):
                ps = mk.nextP()
                for kc in range(2):
                    kb.op("pe", lambda e, ps=ps, kc=kc, hv=hv, tt=tt: e.matmul(
                        ps[:], lhsT=ckvT[:, kc, tt * 128:(tt + 1) * 128],
                        rhs=wkv_v[:, kc, hv * 4:(hv + 1) * 4, :].rearrange("p h d -> p (h d)"),
                        start=(kc == 0), stop=(kc == 1)), [wkv_v, ckvT], [ps])
                st = stg[si % 3]; si += 1
                mk.evac(st[:], ps[:], [ps], [st])
                r0 = jk * 512 + tt * 128
                kb.dma("sp", VT["mv"][r0:r0 + 128, hv * 512:(hv + 1) * 512], st[:], reads=[st], writes=[VT["mv"]])
    mk.end_stage(m0)


QS_MLA = 192 ** -0.5


def stage_B_mla(mk, l, xT):
    kb = mk.kb
    m0 = kb.mark()
    W = mk.W[("w_in", l)]
    Wq = mk.W[("mla_w_q_up", l)]
    QT = mk.QT = getattr(mk, "QT", {})
    for h in range(8):
        QT["mqn%d" % h] = kb.dram("QT_mqn%d_%d" % (h, l), [128, T], BF16)
    QT["mqr"] = kb.dram("QT_mqr_%d" % l, [64, 8 * T], BF16)
    wcq = kb.sb("wcq", [128, 16, 512], BF16)
    kb.dma("sp", wcq[:], W.ap().rearrange("(kc p) n -> p kc n", p=128)[:, :, 0:512], reads=[W], writes=[wcq])
    wq_n = kb.sb("wq_n", [128, 4, 8, 128], BF16)
    wq_r = kb.sb("wq_r", [128, 4, 8, 64], BF16)
    wqv = Wq.ap().rearrange("(kc p) (h d) -> p kc h d", p=128, d=192)
    for kc in range(4):
        kb.dma("sp", wq_n[:, kc], wqv[:, kc, :, 0:128], reads=[Wq], writes=[wq_n])
        kb.dma("sp", wq_r[:, kc], wqv[:, kc, :, 128:192], reads=[Wq], writes=[wq_r])
    gq = kb.sb("gq", [128, 512], F32)
    kb.dma("sp", gq[:], mk.v_in["mla_q_norm"][l:l + 1, :].partition_broadcast(128), reads=[mk.v_in["mla_q_norm"]], writes=[gq])
    cqT = kb.sb("cqT", [128, 4, 512], BF16)
    stg = [kb.sb("stgB%d" % i, [128, 512], BF16) for i in range(3)]
    qrT = [kb.sb("qrT%d" % i, [64, 8, 128], BF16) for i in range(2)]
    si = 0
    for i in range(NQC):
        for tt in range(4):
            t0 = i * 512 + tt * 128
            ps = mk.nextP()
            for kc in range(16):
                kb.op("pe", lambda e, ps=ps, kc=kc, t0=t0: e.matmul(ps[:], lhsT=xT[:, kc, t0:t0 + 128], rhs=wcq[:, kc, :],
                                                                    start=(kc == 0), stop=(kc == 15)), [xT, wcq], [ps])
            cq = mk.tmp("cq_f", [128, 512], F32)
            mk.evac(cq[:], ps[:], [ps], [cq])
            cqn = mk.tmp("cqn", [128, 512], BF16)
            rmsnorm_rows(mk, cq, 512, gq, cqn, 0)
            for k in range(4):
                kb.op("pe", lambda e, k=k, cqn=cqn: e.transpose(out=mk.PT[:, k * 128:(k + 1) * 128], in_=cqn[:, k * 128:(k + 1) * 128],
                                                                identity=mk.c["ident"][:]), [cqn, mk.c["ident"]], [mk.PT])
            mk.evac(cqT[:, :, tt * 128:(tt + 1) * 128], mk.PT[:, 0:512].rearrange("p (k t) -> p k t", k=4), [mk.PT], [cqT])
        for h in range(8):
            ps = mk.nextP()
            for kc in range(4):
                kb.op("pe", lambda e, ps=ps, kc=kc, h=h: e.matmul(ps[:], lhsT=wq_n[:, kc, h, :], rhs=cqT[:, kc, :],
                                                                  start=(kc == 0), stop=(kc == 3)), [wq_n, cqT], [ps])
            st = stg[si % 3]; si += 1
            mk.evac(st[:], ps[:], [ps], [st], scale=QS_MLA)
            kb.dma("sp", QT["mqn%d" % h][:, i * 512:(i + 1) * 512], st[:], reads=[st], writes=[QT["mqn%d" % h]])
        for tt in range(4):
            t0 = i * 512 + tt * 128
            ps = mk.nextP()
            for kc in range(4):
                kb.op("pe", lambda e, ps=ps, kc=kc, tt=tt: e.matmul(
                    ps[:], lhsT=cqT[:, kc, tt * 128:(tt + 1) * 128], rhs=wq_r[:, kc, :, :].rearrange("p h d -> p (h d)"),
                    start=(kc == 0), stop=(kc == 3)), [wq_r, cqT], [ps])
            cs = mk.tmp("csB", [128, 64], F32)
            kb.dma("sp", cs[:], mk.t_rope_own[t0:t0 + 128, :], reads=[mk.t_rope_own], writes=[cs])
            qr = mk.tmp("qrB", [128, 8, 64], BF16)
            for h in range(8):
                rope_rows(mk, ps[:, h * 64:h * 64 + 32], ps[:, h * 64 + 32:h * 64 + 64], cs, qr[:, h, 0:32], qr[:, h, 32:64],
                          [ps], [qr], scale=QS_MLA)
            for h in range(8):
                kb.op("pe", lambda e, h=h, qr=qr: e.transpose(out=mk.PT[0:64, h * 128:(h + 1) * 128], in_=qr[:, h, :],
                                                              identity=mk.c["ident"][:]), [qr, mk.c["ident"]], [mk.PT])
            qt = qrT[(i * 4 + tt) % 2]
            mk.evac(qt[:], mk.PT[0:64, :].rearrange("p (h t) -> p h t", h=8), [mk.PT], [qt])
            kb.dma("sp", QT["mqr"].ap().rearrange("p (h t) -> p h t", h=8)[:, :, t0:t0 + 128], qt[:], reads=[qt], writes=[QT["mqr"]])
    mk.end_stage(m0)


def mla_attention(mk, l, OT):
    kb = mk.kb
    m0 = kb.mark()
    KT, VT, QT = mk.KT, mk.VT, mk.QT
    krT = kb.sb("krT", [64, S], BF16)
    kb.dma("sp", krT[:], KT["mkr"].ap(), reads=[KT["mkr"]], writes=[krT])
    mI = kb.sb("mI", [128, NQC * 8, 512], BF16)
    kb.dma("sp", mI[:], mk.d_mask["I"].ap().rearrange("(n p) t -> p n t", p=128), reads=[mk.d_mask["I"]], writes=[mI])
    knT = [kb.sb("knT%d" % i, [128, S], BF16) for i in range(2)]
    vau = [kb.sb("vau%d" % i, [128, 32, 132], BF16) for i in range(2)]
    for v in vau:
        kb.op("dve", lambda e, v=v: e.memset(v[:, :, 128:132], 1.0), [], [v])
    qn = [kb.sb("qn%d" % i, [128, 512], BF16) for i in range(2)]
    qr = [kb.sb("qr%d" % i, [64, 8, 512], BF16) for i in range(2)]
    eb = [kb.sb("eb%d" % i, [128, 512], BF16) for i in range(3)]
    ob = [kb.sb("ob%d" % i, [128, 128], BF16) for i in range(2)]
    ot = [kb.sb("ot%d" % i, [128, 512], BF16) for i in range(2)]
    rz = kb.sb("rz", [128, 8], F32)
    ei = 0
    oi = 0
    for i in range(NQC):
        q_r = qr[i % 2]
        kb.dma("sp", q_r[:], QT["mqr"].ap().rearrange("p (h t) -> p h t", h=8)[:, :, i * 512:(i + 1) * 512],
               reads=[QT["mqr"]], writes=[q_r])
        nkt = 4 * (JMAX[i] + 1)
        for h in range(8):
            kn, va = knT[h % 2], vau[h % 2]
            kb.dma("sp", kn[:, 0:nkt * 128], KT["mkn%d" % h][:, 0:nkt * 128], reads=[KT["mkn%d" % h]], writes=[kn])
            kb.dma("sp", va[:, 0:nkt, 0:128],
                   VT["mv"].ap().rearrange("(kt p) d -> p kt d", p=128)[:, 0:nkt, h * 128:(h + 1) * 128],
                   reads=[VT["mv"]], writes=[va])
            q_n = qn[h % 2]
            kb.dma("sp", q_n[:], QT["mqn%d" % h][:, i * 512:(i + 1) * 512], reads=[QT["mqn%d" % h]], writes=[q_n])
            for kt in range(nkt):
                ps = mk.nextP()
                msk = kt >= nkt - 8
                kb.op("pe", lambda e, ps=ps, kn=kn, q_n=q_n, kt=kt: e.matmul(
                    ps[:], lhsT=kn[:, kt * 128:(kt + 1) * 128], rhs=q_n[:], start=True, stop=False), [kn, q_n], [ps])
                kb.op("pe", lambda e, ps=ps, q_r=q_r, kt=kt, h=h, msk=msk: e.matmul(
                    ps[:], lhsT=krT[:, kt * 128:(kt + 1) * 128], rhs=q_r[:, h, :], start=False, stop=(not msk)), [krT, q_r], [ps])
                if msk:
                    kb.op("pe", lambda e, ps=ps, i=i, kt=kt, nkt=nkt: e.matmul(
                        ps[:], lhsT=mk.c["ident"][:], rhs=mI[:, i * 8 + kt - (nkt - 8), :], start=False, stop=True),
                        [mk.c["ident"], mI], [ps])
                E = eb[ei % 3]; ei += 1
                kb.op("act", lambda e, E=E, ps=ps: e.activation(out=E[:], in_=ps[:], func=AF.Exp), [ps], [E])
                for c in range(4):
                    kb.op("pe", lambda e, E=E, va=va, c=c, kt=kt, nkt=nkt: e.matmul(
                        mk.PO[c][:, 0:129], lhsT=E[:, c * 128:(c + 1) * 128], rhs=va[:, kt, 0:129],
                        start=(kt == 0), stop=(kt == nkt - 1)), [E, va], [mk.PO[c]])
            o_t = ot[oi % 2]; oi += 1
            for c in range(4):
                o_b = ob[c % 2]
                kb.op("dve", lambda e, c=c: e.reciprocal(out=rz[:, c:c + 1], in_=mk.PO[c][:, 128:129]), [mk.PO[c], rz], [rz])
                kb.op("dve", lambda e, c=c, o_b=o_b: e.tensor_scalar_mul(out=o_b[:], in0=mk.PO[c][:, 0:128], scalar1=rz[:, c:c + 1]),
                      [mk.PO[c], rz], [o_b])
                kb.op("pe", lambda e, o_b=o_b, c=c: e.transpose(out=mk.PT[:, c * 128:(c + 1) * 128], in_=o_b[:],
                                                                identity=mk.c["ident"][:]), [o_b, mk.c["ident"]], [mk.PT])
            mk.evac(o_t[:], mk.PT[:, 0:512], [mk.PT], [o_t])
            kb.dma("sp", OT[h * 128:(h + 1) * 128, i * 512:(i + 1) * 512], o_t[:], reads=[o_t], writes=[OT])
    mk.end_stage(m0)


QS = 128 ** -0.5


def stage_B_rest(mk, l, xT, gates):
    kb = mk.kb
    m0 = kb.mark()
    W = mk.W[("w_in", l)]
    QT = mk.QT
    for h in range(8):
        QT["nq%d" % h] = kb.dram("QT_nq%d_%d" % (h, l), [128, T], BF16)
        QT["sbq%d" % h] = kb.dram("QT_sbq%d_%d" % (h, l), [128, T], BF16)
    wsrc = W.ap().rearrange("(kc p) n -> p kc n", p=128)
    wts = [kb.sb("wB%d" % i, [128, 16, 512], BF16) for i in range(2)]
    stg = [kb.sb("stgR%d" % i, [128, 512], BF16) for i in range(3)]
    wg = kb.sb("wgate", [128, 16, 24], BF16)
    kb.dma("sp", wg[:], wsrc[:, :, OFF_NG:OFF_NG + 24], reads=[W], writes=[wg])
    groups = [(OFF_NQ, ["nq0", "nq1", "nq2", "nq3"]), (OFF_NQ + 512, ["nq4", "nq5", "nq6", "nq7"]),
              (OFF_SB, ["sbq0", "sbq1", "sbq2", "sbq3"]), (OFF_SB + 512, ["sbq4", "sbq5", "sbq6", "sbq7"])]
    wi = si = 0
    for c0, names in groups:
        wt = wts[wi % 2]; wi += 1
        kb.dma("sp", wt[:], wsrc[:, :, c0:c0 + 512], reads=[W], writes=[wt])
        for i in range(NQC):
            for sc, nm in enumerate(names):
                ps = mk.nextP()
                for kc in range(16):
                    kb.op("pe", lambda e, ps=ps, wt=wt, kc=kc, sc=sc, i=i: e.matmul(
                        ps[:], lhsT=wt[:, kc, sc * 128:(sc + 1) * 128], rhs=xT[:, kc, i * 512:(i + 1) * 512],
                        start=(kc == 0), stop=(kc == 15)), [wt, xT], [ps])
                st = stg[si % 3]; si += 1
                mk.evac(st[:], ps[:], [ps], [st], scale=QS)
                kb.dma("sp", QT[nm][:, i * 512:(i + 1) * 512], st[:], reads=[st], writes=[QT[nm]])
    for tt in range(16):
        ps = mk.nextP()
        for kc in range(16):
            kb.op("pe", lambda e, ps=ps, kc=kc, tt=tt: e.matmul(ps[:, 0:24], lhsT=xT[:, kc, tt * 128:(tt + 1) * 128], rhs=wg[:, kc, :],
                                                                start=(kc == 0), stop=(kc == 15)), [wg, xT], [ps])
        kb.op("act", lambda e, ps=ps, tt=tt: e.activation(out=gates[:, tt, :], in_=ps[:, 0:24], func=AF.Sigmoid), [ps], [gates])
    mk.end_stage(m0)


def sb_attention(mk, l, OT):
    kb = mk.kb
    m0 = kb.mark()
    KT, VT, QT = mk.KT, mk.VT, mk.QT
    tri = mk.c["tri"]
    mS = kb.sb("mS", [128, NQC * 8, 512], BF16)
    kb.dma("sp", mS[:], mk.d_mask["S"].ap().rearrange("(n p) t -> p n t", p=128), reads=[mk.d_mask["S"]], writes=[mS])
    kT = [kb.sb("sbk%d" % i, [128, S], BF16) for i in range(2)]
    vv = [kb.sb("sbv%d" % i, [128, 32, 128], BF16) for i in range(2)]
    qq = [kb.sb("sbq%d" % i, [128, 512], BF16) for i in range(2)]
    ef = [kb.sb("sbe%d" % i, [128, 512], F32) for i in range(2)]
    sp = [kb.sb("sbsp%d" % i, [128, 512], BF16) for i in range(2)]
    tm = [kb.sb("sbtm%d" % i, [128, 512], F32) for i in range(2)]
    ab = [kb.sb("sba%d" % i, [128, 512], BF16) for i in range(2)]
    carry = kb.sb("sbcarry", [128, 512], F32)
    ob = [kb.sb("sbob%d" % i, [128, 128], BF16) for i in range(2)]
    ot = [kb.sb("sbot%d" % i, [128, 512], BF16) for i in range(2)]
    it = 0
    hi = 0
    for i in range(NQC):
        nkt = 4 * (JMAX[i] + 1)
        for h in range(8):
            kt_, v_, q_ = kT[hi % 2], vv[hi % 2], qq[hi % 2]
            hi += 1
            kb.dma("sp", kt_[:, 0:nkt * 128], KT["sbk%d" % h][:, 0:nkt * 128], reads=[KT["sbk%d" % h]], writes=[kt_])
            kb.dma("sp", v_[:, 0:nkt, :], VT["sbv"].ap().rearrange("(kt p) d -> p kt d", p=128)[:, 0:nkt, h * 128:(h + 1) * 128],
                   reads=[VT["sbv"]], writes=[v_])
            kb.dma("sp", q_[:], QT["sbq%d" % h][:, i * 512:(i + 1) * 512], reads=[QT["sbq%d" % h]], writes=[q_])
            kb.op("dve", lambda e: e.memset(carry[:], 0.0), [], [carry])
            for kt in range(nkt - 1, -1, -1):
                e_, sp_, tm_, a_ = ef[it % 2], sp[it % 2], tm[it % 2], ab[it % 2]
                it += 1
                msk = kt >= nkt - 8
                pz = mk.nextP()
                kb.op("pe", lambda e, pz=pz, kt_=kt_, q_=q_, kt=kt, msk=msk: e.matmul(
                    pz[:], lhsT=kt_[:, kt * 128:(kt + 1) * 128], rhs=q_[:], start=True, stop=(not msk)), [kt_, q_], [pz])
                if msk:
                    kb.op("pe", lambda e, pz=pz, i=i, kt=kt, nkt=nkt: e.matmul(
                        pz[:], lhsT=mk.c["ident"][:], rhs=mS[:, i * 8 + kt - (nkt - 8), :], start=False, stop=True),
                        [mk.c["ident"], mS], [pz])
                kb.op("act", lambda e, e_=e_, pz=pz: e.activation(out=e_[:], in_=pz[:], func=AF.Exp), [pz], [e_])
                kb.op("act", lambda e, e_=e_, sp_=sp_: e.activation(out=sp_[:], in_=e_[:], func=AF.Ln, bias=mk.c["eps"][:, 2:3]),
                      [e_, mk.c["eps"]], [sp_])
                pc = mk.nextP()
                kb.op("pe", lambda e, pc=pc, sp_=sp_: e.matmul(pc[:], lhsT=tri[:, 0:128], rhs=sp_[:], start=True, stop=True), [tri, sp_], [pc])
                po = mk.nextP()
                kb.op("pe", lambda e, po=po, sp_=sp_: e.matmul(po[:], lhsT=tri[:, 128:256], rhs=sp_[:], start=True, stop=True), [tri, sp_], [po])
                kb.op("dve", lambda e, tm_=tm_, pc=pc: e.tensor_tensor(out=tm_[:], in0=pc[:], in1=carry[:], op=ALU.add), [pc, carry], [tm_])
                kb.op("dve", lambda e, tm_=tm_, pz=pz: e.tensor_tensor(out=tm_[:], in0=pz[:], in1=tm_[:], op=ALU.subtract), [pz, tm_], [tm_])
                kb.op("act", lambda e, a_=a_, tm_=tm_: e.activation(out=a_[:], in_=tm_[:], func=AF.Exp), [tm_], [a_])
                kb.op("dve", lambda e, po=po: e.tensor_tensor(out=carry[:], in0=po[:], in1=carry[:], op=ALU.add), [po, carry], [carry])
                for c in range(4):
                    kb.op("pe", lambda e, a_=a_, v_=v_, c=c, kt=kt, nkt=nkt: e.matmul(
                        mk.PO[c][:, 0:128], lhsT=a_[:, c * 128:(c + 1) * 128], rhs=v_[:, kt, :],
                        start=(kt == nkt - 1), stop=(kt == 0)), [a_, v_], [mk.PO[c]])
            o_t = ot[hi % 2]
            for c in range(4):
                o_b = ob[c % 2]
                mk.evac(o_b[:], mk.PO[c][:, 0:128], [mk.PO[c]], [o_b])
                kb.op("pe", lambda e, o_b=o_b, c=c: e.transpose(out=mk.PT[:, c * 128:(c + 1) * 128], in_=o_b[:],
                                                                identity=mk.c["ident"][:]), [o_b, mk.c["ident"]], [mk.PT])
            mk.evac(o_t[:], mk.PT[:, 0:512], [mk.PT], [o_t])
            kb.dma("sp", OT[2048 + h * 128:2048 + (h + 1) * 128, i * 512:(i + 1) * 512], o_t[:], reads=[o_t], writes=[OT])
    mk.end_stage(m0)


def nsa_compress(mk, l, kcmpT, vaug):
    kb = mk.kb
    m0 = kb.mark()
    src = [kb.sb("cmpsrc%d" % i, [128, S], BF16) for i in range(2)]
    w1 = [kb.sb("cmpw1%d" % i, [128, 32, 128], BF16) for i in range(2)]
    w2 = [kb.sb("cmpw2%d" % i, [128, 128], BF16) for i in range(2)]
    pef = kb.sb("cmppef", [32, 128], F32)
    pe32 = kb.sb("cmppe32", [32, 128], BF16)
    peb = kb.sb("cmppeb", [128, 32], BF16)
    c1 = kb.sb("cmpc1", [128, 2], F32)
    xs = kb.sb("cmpx", [128, 256], F32)
    us = kb.sb("cmpu", [128, 256], F32)
    sg = kb.sb("cmps", [128, 256], F32)
    h1 = kb.sb("cmph1", [128, 256], BF16)
    kb.op("dve", lambda e: e.memset(h1[:], 0.0), [], [h1])
    n = 0
    for kv in range(2):
        W1 = mk.W[("nsa_w1_k" if kv == 0 else "nsa_w1_v", l)]
        W2 = mk.W[("nsa_w2_k" if kv == 0 else "nsa_w2_v", l)]
        pev = mk.v_in["nsa_pe_k" if kv == 0 else "nsa_pe_v"]
        w1s, w2s = w1[kv], w2[kv]
        kb.dma("sp", w1s[:], W1.ap().rearrange("(l d) j -> d l j", d=128), reads=[W1], writes=[w1s])
        kb.dma("sp", w2s[:], W2.ap(), reads=[W2], writes=[w2s])
        kb.dma("sp", pef[0:32, :], pev[l:l + 1, :].rearrange("o (l d) -> (o l) d", d=128), reads=[pev], writes=[pef])
        kb.op("dve", lambda e: e.tensor_copy(out=pe32[0:32, :], in_=pef[0:32, :]), [pef], [pe32])
        kb.op("pe", lambda e: e.transpose(out=mk.PT[:, 0:32], in_=pe32[0:32, :], identity=mk.c["ident"][0:32, 0:32]),
              [pe32, mk.c["ident"]], [mk.PT])
        mk.evac(peb[:], mk.PT[:, 0:32], [mk.PT], [peb])
        pc = mk.nextP()
        for ll in range(32):
            kb.op("pe", lambda e, pc=pc, ll=ll, w1s=w1s: e.matmul(pc[:, 0:1], lhsT=w1s[:, ll, :], rhs=peb[:, ll:ll + 1],
                                                                  start=(ll == 0), stop=(ll == 31)), [w1s, peb], [pc])
        kb.op("dve", lambda e, pc=pc: e.tensor_copy(out=c1[:, 0:1], in_=pc[:, 0:1]), [pc], [c1])
        for g in range(2):
            s_ = src[n % 2]; n += 1
            nm = "n%d" % (kv * 2 + g)
            kb.dma("sp", s_[:], mk.KT[nm].ap(), reads=[mk.KT[nm]], writes=[s_])
            ps = mk.nextP()
            for ll in range(32):
                kb.op("pe", lambda e, ps=ps, ll=ll, w1s=w1s, s_=s_: e.matmul(
                    ps[:, 0:255], lhsT=w1s[:, ll, :], rhs=s_[:, ll:ll + 16 * 254 + 1:16], start=(ll == 0), stop=(ll == 31)),
                    [w1s, s_], [ps])
            kb.op("dve", lambda e, ps=ps: e.tensor_scalar_add(out=xs[:, 0:255], in0=ps[:, 0:255], scalar1=c1[:, 0:1]), [ps, c1], [xs])
            kb.op("dve", lambda e: e.tensor_tensor(out=us[:, 0:255], in0=xs[:, 0:255], in1=xs[:, 0:255], op=ALU.mult), [xs], [us])
            kb.op("dve", lambda e: e.tensor_scalar(out=us[:, 0:255], in0=us[:, 0:255], scalar1=0.044715, scalar2=1.0,
                                                   op0=ALU.mult, op1=ALU.add), [us], [us])
            kb.op("dve", lambda e: e.tensor_tensor(out=us[:, 0:255], in0=us[:, 0:255], in1=xs[:, 0:255], op=ALU.mult), [us, xs], [us])
            kb.op("act", lambda e: e.activation(out=sg[:, 0:255], in_=us[:, 0:255], func=AF.Sigmoid, scale=1.5957691216057308), [us], [sg])
            kb.op("dve", lambda e: e.tensor_tensor(out=h1[:, 0:255], in0=xs[:, 0:255], in1=sg[:, 0:255], op=ALU.mult), [xs, sg], [h1])
            if kv == 0:
                p2 = mk.nextP()
                kb.op("pe", lambda e, p2=p2, w2s=w2s: e.matmul(p2[:, 0:256], lhsT=w2s[:], rhs=h1[:], start=True, stop=True), [w2s, h1], [p2])
                mk.evac(kcmpT[g][:], p2[:, 0:256], [p2], [kcmpT[g]])
            else:
                for nq in range(2):
                    p2 = mk.nextP()
                    kb.op("pe", lambda e, p2=p2, w2s=w2s, nq=nq: e.matmul(p2[:, 0:128], lhsT=h1[:, nq * 128:(nq + 1) * 128], rhs=w2s[:],
                                                                          start=True, stop=True), [w2s, h1], [p2])
                    mk.evac(vaug[g][:, nq, 0:128], p2[:, 0:128], [p2], [vaug[g]])
    for g in range(2):
        kb.op("dve", lambda e, g=g: e.memset(vaug[g][:, :, 128:129], 1.0), [vaug[g]], [vaug[g]])
        kb.dma("sp", vaug[g][:, :, 129:193], mk.d_ov.ap().rearrange("(q p) j -> p q j", p=128), reads=[mk.d_ov], writes=[vaug[g]])
    mk.end_stage(m0)


def nsa_attention(mk, l, OT, gates):
    kb = mk.kb
    mP = kb.mark()
    kcmpT = [kb.sb("kcmpT%d" % g, [128, 256], BF16) for g in range(2)]
    vaug = [kb.sb("vcaug%d" % g, [128, 2, 196], BF16) for g in range(2)]
    nsa_compress(mk, l, kcmpT, vaug)
    m0 = kb.mark()
    KT, VT, QT = mk.KT, mk.VT, mk.QT
    c = mk.c
    et = kb.sb("et", [64, S], BF16)
    kb.dma("sp", et[:], mk.d_et.ap(), reads=[mk.d_et], writes=[et])
    c["bcol"] = kb.sb("bcol", [128, NQC * 32 * 8], F32)
    kb.dma("sp", c["bcol"][:], mk.t_bcol.ap(), reads=[mk.t_bcol], writes=[c["bcol"]])
    mC = kb.sb("mC", [128, NQC * 2, 512], BF16)
    kb.dma("sp", mC[:], mk.d_mask["C"].ap().rearrange("(n p) t -> p n t", p=128), reads=[mk.d_mask["C"]], writes=[mC])
    mI = kb.sb("mIn", [128, 8, 512], BF16)
    mW = kb.sb("mWn", [128, 12, 512], BF16)
    selk = kb.sb("selk", [128, S], BF16)
    wink = kb.sb("wink", [128, 12 * 128], BF16)
    selv = kb.sb("selv", [128, 32, 132], BF16)
    winv = kb.sb("winv", [128, 12, 132], BF16)
    kb.op("dve", lambda e: e.memset(selv[:, :, 128:132], 1.0), [], [selv])
    kb.op("dve", lambda e: e.memset(winv[:, :, 128:132], 1.0), [], [winv])
    qq = [kb.sb("nq%d" % i, [128, 512], BF16) for i in range(2)]
    eb = [kb.sb("neb%d" % i, [128, 512], BF16) for i in range(3)]
    onsa = kb.sb("onsa", [128, 4, 4, 128], F32)
    imp = kb.sb("imp", [128, 4, 64], F32)
    fb = kb.sb("fbt", [128, 4, 64], F32)
    rz = kb.sb("nrz", [128, 4], F32)
    cf = kb.sb("ncf", [128, 4], F32)
    m8 = kb.sb("m8", [128, 16], F32)
    wk = kb.sb("tkw", [128, 64], F32)
    Mf = kb.sb("Mf", [128, 64], F32)
    Mb = kb.sb("Mb", [128, 64], BF16)
    negmT = kb.sb("negmT", [64, 512], BF16)
    ob = [kb.sb("nob%d" % i, [128, 128], BF16) for i in range(2)]
    ot = [kb.sb("not%d" % i, [128, 512], BF16) for i in range(2)]
    ei = [0]
    qi = [0]

    def finish_branch(h, hl, br, first):
        for cc in range(4):
            P_ = mk.PO[cc]
            kb.op("dve", lambda e, P_=P_, cc=cc: e.tensor_scalar_max(out=rz[:, cc:cc + 1], in0=P_[:, 128:129], scalar1=1e-30), [P_, rz], [rz])
            kb.op("dve", lambda e, cc=cc: e.reciprocal(out=rz[:, cc:cc + 1], in_=rz[:, cc:cc + 1]), [rz], [rz])
            if br == 0:
                if first:
                    kb.op("dve", lambda e, P_=P_, cc=cc: e.tensor_scalar_mul(out=imp[:, cc, :], in0=P_[:, 129:193], scalar1=rz[:, cc:cc + 1]),
                          [P_, rz], [imp])
                else:
                    kb.op("dve", lambda e, P_=P_, cc=cc: e.scalar_tensor_tensor(out=imp[:, cc, :], in0=P_[:, 129:193], scalar=rz[:, cc:cc + 1],
                                                                                in1=imp[:, cc, :], op0=ALU.mult, op1=ALU.add), [P_, rz, imp], [imp])
            return_tile = None
        return return_tile

    for i in range(NQC):
        nkt = 4 * (JMAX[i] + 1)
        kb.dma("sp", mI[:], mk.d_mask["I"].ap().rearrange("(n p) t -> p n t", p=128)[:, i * 8:(i + 1) * 8, :], reads=[mk.d_mask["I"]], writes=[mI])
        kb.dma("sp", mW[:], mk.d_mask["W"].ap().rearrange("(n p) t -> p n t", p=128)[:, i * 12:(i + 1) * 12, :], reads=[mk.d_mask["W"]], writes=[mW])
        kb.dma("sp", fb[:], mk.t_fb.ap().rearrange("(c p) j -> p c j", p=128)[:, i * 4:(i + 1) * 4, :], reads=[mk.t_fb], writes=[fb])
        wlo = max(0, nkt - 12)
        for g in range(2):
            kb.dma("sp", selk[:, 0:nkt * 128], KT["n%d" % (4 + g)][:, 0:nkt * 128], reads=[KT["n%d" % (4 + g)]], writes=[selk])
            kb.dma("sp", wink[:, 0:(nkt - wlo) * 128], KT["n%d" % (8 + g)][:, wlo * 128:nkt * 128], reads=[KT["n%d" % (8 + g)]], writes=[wink])
            kb.dma("sp", selv[:, 0:nkt, 0:128], VT["selv"].ap().rearrange("(kt p) d -> p kt d", p=128)[:, 0:nkt, g * 128:(g + 1) * 128],
                   reads=[VT["selv"]], writes=[selv])
            kb.dma("sp", winv[:, 0:nkt - wlo, 0:128], VT["winv"].ap().rearrange("(kt p) d -> p kt d", p=128)[:, wlo:nkt, g * 128:(g + 1) * 128],
                   reads=[VT["winv"]], writes=[winv])
            qs = []
            for hl in range(4):
                h = g * 4 + hl
                q_ = qq[qi[0] % 2]; qi[0] += 1
                kb.dma("sp", q_[:], QT["nq%d" % h][:, i * 512:(i + 1) * 512], reads=[QT["nq%d" % h]], writes=[q_])
                for nq in range(2):
                    ps = mk.nextP()
                    kb.op("pe", lambda e, ps=ps, g=g, nq=nq, q_=q_: e.matmul(ps[:], lhsT=kcmpT[g][:, nq * 128:(nq + 1) * 128], rhs=q_[:],
                                                                             start=True, stop=False), [kcmpT[g], q_], [ps])
                    kb.op("pe", lambda e, ps=ps, h=h: e.matmul(ps[:], lhsT=c["alck"][0:4, h * 128:(h + 1) * 128],
                                                               rhs=c["alq"][0:4, h * 512:(h + 1) * 512], start=False, stop=False),
                          [c["alck"], c["alq"]], [ps])
                    kb.op("pe", lambda e, ps=ps, i=i, nq=nq: e.matmul(ps[:], lhsT=c["ident"][:], rhs=mC[:, i * 2 + nq, :], start=False, stop=True),
                          [c["ident"], mC], [ps])
                    E = eb[ei[0] % 3]; ei[0] += 1
                    col = (i * 2 + nq) * 8 + h
                    kb.op("act", lambda e, E=E, ps=ps, col=col: e.activation(out=E[:], in_=ps[:], func=AF.Exp, bias=c["bcolc"][:, col:col + 1]),
                          [ps, c["bcolc"]], [E])
                    for cc in range(4):
                        kb.op("pe", lambda e, E=E, g=g, nq=nq, cc=cc: e.matmul(mk.PO[cc][:, 0:193], lhsT=E[:, cc * 128:(cc + 1) * 128],
                                                                              rhs=vaug[g][:, nq, 0:193], start=(nq == 0), stop=(nq == 1)),
                              [E, vaug[g]], [mk.PO[cc]])
                for cc in range(4):
                    P_ = mk.PO[cc]
                    tt = i * 4 + cc
                    kb.op("dve", lambda e, P_=P_, cc=cc: e.tensor_scalar_max(out=rz[:, cc:cc + 1], in0=P_[:, 128:129], scalar1=1e-30), [P_, rz], [rz])
                    kb.op("dve", lambda e, cc=cc: e.reciprocal(out=rz[:, cc:cc + 1], in_=rz[:, cc:cc + 1]), [rz], [rz])
                    if hl == 0:
                        kb.op("dve", lambda e, P_=P_, cc=cc: e.tensor_scalar_mul(out=imp[:, cc, :], in0=P_[:, 129:193], scalar1=rz[:, cc:cc + 1]),
                              [P_, rz], [imp])
                    else:
                        kb.op("dve", lambda e, P_=P_, cc=cc: e.scalar_tensor_tensor(out=imp[:, cc, :], in0=P_[:, 129:193], scalar=rz[:, cc:cc + 1],
                                                                                    in1=imp[:, cc, :], op0=ALU.mult, op1=ALU.add), [P_, rz, imp], [imp])
                    kb.op("dve", lambda e, cc=cc, tt=tt, h=h: e.tensor_tensor(out=cf[:, cc:cc + 1], in0=rz[:, cc:cc + 1],
                                                                              in1=gates[:, tt, h * 3:h * 3 + 1], op=ALU.mult), [rz, gates], [cf])
                    kb.op("dve", lambda e, P_=P_, cc=cc, hl=hl: e.tensor_scalar_mul(out=onsa[:, hl, cc, :], in0=P_[:, 0:128], scalar1=cf[:, cc:cc + 1]),
                          [P_, cf], [onsa])
            for cc in range(4):
                kb.op("dve", lambda e, cc=cc: e.tensor_tensor(out=wk[:], in0=imp[:, cc, :], in1=fb[:, cc, :], op=ALU.add), [imp, fb], [wk])
                kb.op("dve", lambda e: e.max(out=m8[:, 0:8], in_=wk[:]), [wk], [m8])
                kb.op("dve", lambda e: e.match_replace(out=Mf[:], in_to_replace=m8[:, 0:8], in_values=wk[:], imm_value=-1e30), [m8, wk], [Mf])
                kb.op("dve", lambda e: e.max(out=m8[:, 8:16], in_=Mf[:]), [Mf], [m8])
                kb.op("dve", lambda e: e.tensor_scalar(out=Mf[:], in0=wk[:], scalar1=m8[:, 15:16], scalar2=None, op0=ALU.is_ge), [wk, m8], [Mf])
                kb.op("dve", lambda e: e.tensor_scalar(out=Mb[:], in0=Mf[:], scalar1=1.0, scalar2=-NEGM, op0=ALU.subtract, op1=ALU.mult),
                      [Mf], [Mb])
                kb.op("pe", lambda e, cc=cc: e.transpose(out=mk.PT[0:64, cc * 128:(cc + 1) * 128], in_=Mb[:], identity=c["ident"][:]),
                      [Mb, c["ident"]], [mk.PT])
            mk.evac(negmT[:], mk.PT[0:64, 0:512], [mk.PT], [negmT])
            for hl in range(4):
                h = g * 4 + hl
                q_ = qq[qi[0] % 2]; qi[0] += 1
                kb.dma("sp", q_[:], QT["nq%d" % h][:, i * 512:(i + 1) * 512], reads=[QT["nq%d" % h]], writes=[q_])
                for br in (1, 2):
                    kts = list(range(nkt)) if br == 1 else list(range(wlo, nkt))
                    for kt in kts:
                        ps = mk.nextP()
                        if br == 1:
                            lk = selk[:, kt * 128:(kt + 1) * 128]; kbuf = selk
                        else:
                            lk = wink[:, (kt - wlo) * 128:(kt - wlo + 1) * 128]; kbuf = wink
                        kb.op("pe", lambda e, ps=ps, lk=lk, q_=q_: e.matmul(ps[:], lhsT=lk, rhs=q_[:], start=True, stop=False), [kbuf, q_], [ps])
                        kb.op("pe", lambda e, ps=ps, h=h: e.matmul(ps[:], lhsT=c["alk"][0:3, h * 128:(h + 1) * 128],
                                                                   rhs=c["alq"][0:3, h * 512:(h + 1) * 512], start=False, stop=False),
                              [c["alk"], c["alq"]], [ps])
                        if br == 1:
                            last = kt < nkt - 8
                            kb.op("pe", lambda e, ps=ps, kt=kt, last=last: e.matmul(ps[:], lhsT=et[:, kt * 128:(kt + 1) * 128], rhs=negmT[:],
                                                                                   start=False, stop=last), [et, negmT], [ps])
                            if not last:
                                kb.op("pe", lambda e, ps=ps, kt=kt, nkt=nkt: e.matmul(ps[:], lhsT=c["ident"][:], rhs=mI[:, kt - (nkt - 8), :],
                                                                                     start=False, stop=True), [c["ident"], mI], [ps])
                        else:
                            kb.op("pe", lambda e, ps=ps, kt=kt, nkt=nkt: e.matmul(ps[:], lhsT=c["ident"][:], rhs=mW[:, kt - (nkt - 12), :],
                                                                                 start=False, stop=True), [c["ident"], mW], [ps])
                        E = eb[ei[0] % 3]; ei[0] += 1
                        col = (i * 32 + kt) * 8 + h
                        kb.op("act", lambda e, E=E, ps=ps, col=col: e.activation(out=E[:], in_=ps[:], func=AF.Exp, bias=c["bcol"][:, col:col + 1]),
                              [ps, c["bcol"]], [E])
                        for cc in range(4):
                            if br == 1:
                                rv = selv[:, kt, 0:129]; vbuf = selv
                            else:
                                rv = winv[:, kt - wlo, 0:129]; vbuf = winv
                            kb.op("pe", lambda e, E=E, rv=rv, cc=cc, kt=kt, k0=kts[0], k1=kts[-1]: e.matmul(
                                mk.PO[cc][:, 0:129], lhsT=E[:, cc * 128:(cc + 1) * 128], rhs=rv, start=(kt == k0), stop=(kt == k1)),
                                [E, vbuf], [mk.PO[cc]])
                    for cc in range(4):
                        P_ = mk.PO[cc]
                        tt = i * 4 + cc
                        kb.op("dve", lambda e, P_=P_, cc=cc: e.tensor_scalar_max(out=rz[:, cc:cc + 1], in0=P_[:, 128:129], scalar1=1e-30), [P_, rz], [rz])
                        kb.op("dve", lambda e, cc=cc: e.reciprocal(out=rz[:, cc:cc + 1], in_=rz[:, cc:cc + 1]), [rz], [rz])
                        kb.op("dve", lambda e, cc=cc, tt=tt, h=h, br=br: e.tensor_tensor(out=cf[:, cc:cc + 1], in0=rz[:, cc:cc + 1],
                                                                                         in1=gates[:, tt, h * 3 + br:h * 3 + br + 1], op=ALU.mult), [rz, gates], [cf])
                        kb.op("dve", lambda e, P_=P_, cc=cc, hl=hl: e.scalar_tensor_tensor(out=onsa[:, hl, cc, :], in0=P_[:, 0:128], scalar=cf[:, cc:cc + 1],
                                                                                           in1=onsa[:, hl, cc, :], op0=ALU.mult, op1=ALU.add),
                              [P_, cf, onsa], [onsa])
                o_t = ot[hl % 2]
                for cc in range(4):
                    o_b = ob[cc % 2]
                    kb.op("dve", lambda e, o_b=o_b, hl=hl, cc=cc: e.tensor_copy(out=o_b[:], in_=onsa[:, hl, cc, :]), [onsa], [o_b])
                    kb.op("pe", lambda e, o_b=o_b, cc=cc: e.transpose(out=mk.PT[:, cc * 128:(cc + 1) * 128], in_=o_b[:], identity=c["ident"][:]),
                          [o_b, c["ident"]], [mk.PT])
                mk.evac(o_t[:], mk.PT[:, 0:512], [mk.PT], [o_t])
                kb.dma("sp", OT[1024 + h * 128:1024 + (h + 1) * 128, i * 512:(i + 1) * 512], o_t[:], reads=[o_t], writes=[OT])
    mk.end_stage(mP)


def ln_tile(mk, z, gB, bB, out_f, junk=None):
    kb = mk.kb
    st = mk.tmp("ln_st", [128, 4], F32)
    if junk is None:
        junk = mk.tmp("ln_junk", [128, D], BF16)
    jap = junk[:, 0:D] if len(junk.h.shape) == 2 else junk[:].rearrange("p a b -> p (a b)")[:, 0:D]
    kb.op("dve", lambda e: e.reduce_sum(out=st[:, 0:1], in_=z[:], axis=AX.X), [z, st], [st])
    kb.op("dve", lambda e: e.tensor_scalar_mul(out=st[:, 0:1], in0=st[:, 0:1], scalar1=1.0 / D), [st], [st])
    kb.op("dve", lambda e: e.tensor_scalar_sub(out=z[:], in0=z[:], scalar1=st[:, 0:1]), [z, st], [z])
    kb.op("dve", lambda e: e.memset(st[:, 1:2], 0.0), [st], [st])
    kb.op("act", lambda e: e.activation(out=jap, in_=z[:], func=AF.Square, accum_out=st[:, 1:2]), [z, st], [junk, st])
    kb.op("act", lambda e: e.activation(out=st[:, 2:3], in_=st[:, 1:2], func=AF.Sqrt, scale=1.0 / D, bias=mk.c["eps"][:, 1:2]),
          [st, mk.c["eps"]], [st])
    kb.op("dve", lambda e: e.reciprocal(out=st[:, 3:4], in_=st[:, 2:3]), [st], [st])
    kb.op("dve", lambda e: e.scalar_tensor_tensor(out=out_f[:], in0=z[:], scalar=st[:, 3:4], in1=gB[:], op0=ALU.mult, op1=ALU.mult),
          [z, st, gB], [out_f])
    kb.op("dve", lambda e: e.tensor_tensor(out=out_f[:], in0=out_f[:], in1=bB[:], op=ALU.add), [out_f, bB], [out_f])


def load_ln_params(mk, l, gname, bname):
    kb = mk.kb
    gB = kb.sb("lng", [128, D], F32)
    bB = kb.sb("lnb", [128, D], F32)
    kb.dma("sp", gB[:], mk.v_in[gname][l:l + 1, :].partition_broadcast(128), reads=[mk.v_in[gname]], writes=[gB])
    kb.dma("sp", bB[:], mk.v_in[bname][l:l + 1, :].partition_broadcast(128), reads=[mk.v_in[bname]], writes=[bB])
    return gB, bB


def stage_E(mk, l, xT, OT, xsrc, xdst):
    kb = mk.kb
    m0 = kb.mark()
    W = mk.W[("w_in", l)]
    Wb = mk.W[("w_branch", l)]
    Wo = mk.W[("w_out", l)]
    gB, bB = load_ln_params(mk, l, "ln_mix_g", "ln_mix_b")
    bmg = kb.sb("bmg", [128, 48], F32)
    bm32 = kb.sb("bm32", [48, 128], F32)
    kb.dma("sp", bm32[:], mk.v_in["b_merge"][l:l + 1, :].rearrange("o (q p) -> (o q) p", p=128), reads=[mk.v_in["b_merge"]], writes=[bm32])
    bm32b = kb.sb("bm32b", [48, 128], BF16)
    kb.op("dve", lambda e: e.tensor_copy(out=bm32b[:], in_=bm32[:]), [bm32], [bm32b])
    kb.op("pe", lambda e: e.transpose(out=mk.PT[:, 0:48], in_=bm32b[:], identity=mk.c["ident"][0:48, 0:48]), [bm32b, mk.c["ident"]], [mk.PT])
    mk.evac(bmg[:], mk.PT[:, 0:48], [mk.PT], [bmg])
    oT = kb.sb("oTE", [128, 24, 512], BF16)
    yT = kb.sb("yTE", [128, 16, 512], BF16)
    wmg = kb.sb("wmgW", [128, 16, 512], BF16)
    wbr = kb.sb("wbrW", [128, 8, 512], BF16)
    wo = [kb.sb("woE%d" % i, [128, 16, 512], BF16) for i in range(1)]
    gT = kb.sb("gTE", [128, 512], F32)
    tA = kb.sb("tAE", [128, 512], F32)
    yA = [kb.sb("yAE%d" % i, [128, 512], F32) for i in range(4)]
    xt = kb.sb("xtE", [128, D], F32)
    zt = kb.sb("ztE", [128, D], F32)
    wmsrc = W.ap().rearrange("(kc p) n -> p kc n", p=128)
    wbsrc = Wb.ap().rearrange("(q p) c -> p q c", p=128)
    wosrc = Wo.ap().rearrange("(kc p) n -> p kc n", p=128)
    wi = 0
    for i in range(NQC):
        kb.dma("sp", oT[:], OT.ap().rearrange("(q p) t -> p q t", p=128)[:, :, i * 512:(i + 1) * 512], reads=[OT], writes=[oT])
        for ccg in range(4):
            for br in range(3):
                c0 = OFF_MG + br * 2048 + ccg * 512
                kb.dma("sp", wmg[:], wmsrc[:, :, c0:c0 + 512], reads=[W], writes=[wmg])
                kb.dma("sp", wbr[:], wbsrc[:, br * 8:(br + 1) * 8, ccg * 512:(ccg + 1) * 512], reads=[Wb], writes=[wbr])
                for c4 in range(4):
                    cc = ccg * 4 + c4
                    pg = mk.nextP()
                    for kc in range(16):
                        kb.op("pe", lambda e, pg=pg, kc=kc, c4=c4, i=i: e.matmul(pg[:], lhsT=wmg[:, kc, c4 * 128:(c4 + 1) * 128],
                                                                                rhs=xT[:, kc, i * 512:(i + 1) * 512],
                                                                                start=(kc == 0), stop=(kc == 15)), [wmg, xT], [pg])
                    col = br * 16 + cc
                    kb.op("act", lambda e, pg=pg, col=col: e.activation(out=gT[:], in_=pg[:], func=AF.Sigmoid, bias=bmg[:, col:col + 1]), [pg, bmg], [gT])
                    pp = mk.nextP()
                    for kc in range(8):
                        kb.op("pe", lambda e, pp=pp, kc=kc, br=br, c4=c4: e.matmul(pp[:], lhsT=wbr[:, kc, c4 * 128:(c4 + 1) * 128], rhs=oT[:, br * 8 + kc, :],
                                                                                  start=(kc == 0), stop=(kc == 7)), [wbr, oT], [pp])
                    y_ = yA[c4]
                    if br == 0:
                        kb.op("dve", lambda e, pp=pp, y_=y_: e.tensor_tensor(out=y_[:], in0=pp[:], in1=gT[:], op=ALU.mult), [pp, gT], [y_])
                    else:
                        kb.op("dve", lambda e, pp=pp: e.tensor_tensor(out=tA[:], in0=pp[:], in1=gT[:], op=ALU.mult), [pp, gT], [tA])
                        if br == 1:
                            kb.op("dve", lambda e, y_=y_: e.tensor_tensor(out=y_[:], in0=y_[:], in1=tA[:], op=ALU.add), [y_, tA], [y_])
                        else:
                            kb.op("dve", lambda e, cc=cc, y_=y_: e.tensor_tensor(out=yT[:, cc, :], in0=y_[:], in1=tA[:], op=ALU.add), [y_, tA], [yT])
        for tt in range(4):
            r0 = i * 512 + tt * 128
            kb.dma("sp", xt[:], xsrc[r0:r0 + 128, :], reads=[xsrc], writes=[xt])
            for n4 in range(4):
                w_ = wo[0]
                kb.dma("sp", w_[:], wosrc[:, :, n4 * 512:(n4 + 1) * 512], reads=[Wo], writes=[w_])
                ps = mk.nextP()
                for kc in range(16):
                    kb.op("pe", lambda e, ps=ps, w_=w_, kc=kc, tt=tt: e.matmul(ps[:], lhsT=yT[:, kc, tt * 128:(tt + 1) * 128], rhs=w_[:, kc, :],
                                                                             start=(kc == 0), stop=(kc == 15)), [w_, yT], [ps])
                kb.op("dve", lambda e, ps=ps, n4=n4: e.scalar_tensor_tensor(out=zt[:, n4 * 512:(n4 + 1) * 512], in0=xt[:, n4 * 512:(n4 + 1) * 512],
                                                                          scalar=DN_ALPHA, in1=ps[:], op0=ALU.mult, op1=ALU.add), [xt, ps], [zt])
            ln_tile(mk, zt, gB, bB, xt, junk=wmg)
            kb.dma("sp", xdst[r0:r0 + 128, :], xt[:], reads=[xt], writes=[xdst])
    mk.end_stage(m0)


def stage_F(mk, l, xT, xsrc, xdst):
    kb = mk.kb
    m0 = kb.mark()
    Wq, Wk, Wv, Wo = (mk.W[(n, l)] for n in ("mem_w_q", "mem_w_k", "mem_w_v", "mem_w_o"))
    gB, bB = load_ln_params(mk, l, "ln_mem_g", "ln_mem_b")
    memT = kb.sb("memT", [128, 16, 256], BF16)
    mf = kb.sb("memf", [128, D], F32)
    mb = kb.sb("memb", [128, D], BF16)
    for mt in range(2):
        kb.dma("sp", mf[:], mk.mem_in[mt * 128:(mt + 1) * 128, :], reads=[mk.mem_in], writes=[mf])
        kb.op("dve", lambda e: e.tensor_copy(out=mb[:], in_=mf[:]), [mf], [mb])
        for g in range(2):
            for k in range(8):
                kc = g * 8 + k
                kb.op("pe", lambda e, kc=kc, k=k: e.transpose(out=mk.PT[:, k * 128:(k + 1) * 128], in_=mb[:, kc * 128:(kc + 1) * 128],
                                                              identity=mk.c["ident"][:]), [mb, mk.c["ident"]], [mk.PT])
            mk.evac(memT[:, g * 8:(g + 1) * 8, mt * 128:(mt + 1) * 128], mk.PT[:, :].rearrange("p (k t) -> p k t", k=8), [mk.PT], [memT])
    wsb = kb.sb("wmemA", [128, 16, 512], BF16)
    kTs = kb.sb("memkT", [128, 4, 256], BF16)
    vau = kb.sb("memv", [128, 2, 4, 132], BF16)
    kb.op("dve", lambda e: e.memset(vau[:, :, :, 128:132], 1.0), [], [vau])
    kb.dma("sp", wsb[:], Wk.ap().rearrange("(kc p) n -> p kc n", p=128), reads=[Wk], writes=[wsb])
    for h in range(4):
        ps = mk.nextP()
        for kc in range(16):
            kb.op("pe", lambda e, ps=ps, kc=kc, h=h: e.matmul(ps[:, 0:256], lhsT=wsb[:, kc, h * 128:(h + 1) * 128], rhs=memT[:, kc, :],
                                                              start=(kc == 0), stop=(kc == 15)), [wsb, memT], [ps])
        mk.evac(kTs[:, h, :], ps[:, 0:256], [ps], [kTs])
    kb.dma("sp", wsb[:], Wv.ap().rearrange("(kc p) n -> p kc n", p=128), reads=[Wv], writes=[wsb])
    for mt in range(2):
        ps = mk.nextP()
        for kc in range(16):
            kb.op("pe", lambda e, ps=ps, kc=kc, mt=mt: e.matmul(ps[:], lhsT=memT[:, kc, mt * 128:(mt + 1) * 128], rhs=wsb[:, kc, :],
                                                                start=(kc == 0), stop=(kc == 15)), [wsb, memT], [ps])
        mk.evac(vau[:, mt, :, 0:128], ps[:].rearrange("p (h d) -> p h d", h=4), [ps], [vau])
    kb.dma("sp", wsb[:], Wq.ap().rearrange("(kc p) n -> p kc n", p=128), reads=[Wq], writes=[wsb])
    wo = kb.sb("wmemO", [128, 4, D], BF16)
    kb.dma("sp", wo[:], Wo.ap().rearrange("(kc p) n -> p kc n", p=128), reads=[Wo], writes=[wo])
    qT = [kb.sb("memq%d" % i, [128, 512], BF16) for i in range(2)]
    eb = [kb.sb("meme%d" % i, [128, 512], BF16) for i in range(3)]
    omT = kb.sb("memoT", [128, 4, 512], BF16)
    ob = [kb.sb("memob%d" % i, [128, 128], BF16) for i in range(2)]
    rz = kb.sb("memrz", [128, 4], F32)
    xt = kb.sb("xtF", [128, D], F32)
    zt = kb.sb("ztF", [128, D], F32)
    ei = 0
    for i in range(NQC):
        for h in range(4):
            q_ = qT[h % 2]
            ps = mk.nextP()
            for kc in range(16):
                kb.op("pe", lambda e, ps=ps, kc=kc, h=h, i=i: e.matmul(ps[:], lhsT=wsb[:, kc, h * 128:(h + 1) * 128], rhs=xT[:, kc, i * 512:(i + 1) * 512],
                                                                      start=(kc == 0), stop=(kc == 15)), [wsb, xT], [ps])
            mk.evac(q_[:], ps[:], [ps], [q_], scale=QS)
            for mt in range(2):
                ps2 = mk.nextP()
                kb.op("pe", lambda e, ps2=ps2, h=h, mt=mt, q_=q_: e.matmul(ps2[:], lhsT=kTs[:, h, mt * 128:(mt + 1) * 128], rhs=q_[:],
                                                                          start=True, stop=True), [kTs, q_], [ps2])
                E = eb[ei % 3]; ei += 1
                kb.op("act", lambda e, E=E, ps2=ps2: e.activation(out=E[:], in_=ps2[:], func=AF.Exp), [ps2], [E])
                for cc in range(4):
                    kb.op("pe", lambda e, E=E, cc=cc, mt=mt, h=h: e.matmul(mk.PO[cc][:, 0:129], lhsT=E[:, cc * 128:(cc + 1) * 128],
                                                                          rhs=vau[:, mt, h, 0:129], start=(mt == 0), stop=(mt == 1)),
                          [E, vau], [mk.PO[cc]])
            for cc in range(4):
                o_b = ob[cc % 2]
                kb.op("dve", lambda e, cc=cc: e.reciprocal(out=rz[:, cc:cc + 1], in_=mk.PO[cc][:, 128:129]), [mk.PO[cc], rz], [rz])
                kb.op("dve", lambda e, cc=cc, o_b=o_b: e.tensor_scalar_mul(out=o_b[:], in0=mk.PO[cc][:, 0:128], scalar1=rz[:, cc:cc + 1]),
                      [mk.PO[cc], rz], [o_b])
                kb.op("pe", lambda e, o_b=o_b, cc=cc: e.transpose(out=mk.PT[:, cc * 128:(cc + 1) * 128], in_=o_b[:], identity=mk.c["ident"][:]),
                      [o_b, mk.c["ident"]], [mk.PT])
            mk.evac(omT[:, h, :], mk.PT[:, 0:512], [mk.PT], [omT])
        for tt in range(4):
            r0 = i * 512 + tt * 128
            kb.dma("sp", xt[:], xsrc[r0:r0 + 128, :], reads=[xsrc], writes=[xt])
            for n4 in range(4):
                ps = mk.nextP()
                for kc in range(4):
                    kb.op("pe", lambda e, ps=ps, kc=kc, tt=tt, n4=n4: e.matmul(ps[:], lhsT=omT[:, kc, tt * 128:(tt + 1) * 128],
                                                                             rhs=wo[:, kc, n4 * 512:(n4 + 1) * 512], start=(kc == 0), stop=(kc == 3)),
                          [wo, omT], [ps])
                kb.op("dve", lambda e, ps=ps, n4=n4: e.scalar_tensor_tensor(out=zt[:, n4 * 512:(n4 + 1) * 512], in0=xt[:, n4 * 512:(n4 + 1) * 512],
                                                                          scalar=DN_ALPHA, in1=ps[:], op0=ALU.mult, op1=ALU.add), [xt, ps], [zt])
            ln_tile(mk, zt, gB, bB, xt)
            kb.dma("sp", xdst[r0:r0 + 128, :], xt[:], reads=[xt], writes=[xdst])
    mk.end_stage(m0)


def stage_G(mk, l, xT, xsrc, xdst):
    kb = mk.kb
    m0 = kb.mark()
    Wr, Wg, Wu, Wd = (mk.W[(n, l)] for n in ("moe_w_router", "moe_w_gate", "moe_w_up", "moe_w_down"))
    gB, bB = load_ln_params(mk, l, "ln_moe_g", "ln_moe_b")
    wr = kb.sb("wrt", [128, 16, 32], BF16)
    kb.dma("sp", wr[:], Wr.ap().rearrange("(kc p) n -> p kc n", p=128), reads=[Wr], writes=[wr])
    brt = kb.sb("brt", [128, 32], F32)
    kb.dma("sp", brt[:], mk.v_in["moe_b_router"][l:l + 1, :].partition_broadcast(128), reads=[mk.v_in["moe_b_router"]], writes=[brt])
    rw = kb.sb("rw", [128, 16, 32], F32)
    rwT = kb.sb("rwT", [32, 16, 128], BF16)
    lg = kb.sb("lg", [128, 32], F32)
    m8 = kb.sb("m8g", [128, 8], F32)
    msk = kb.sb("mskg", [128, 32], F32)
    ex = kb.sb("exg", [128, 32], F32)
    st = kb.sb("stg_", [128, 4], F32)
    rwb = kb.sb("rwb", [128, 32], BF16)
    for tt in range(16):
        ps = mk.nextP()
        for kc in range(16):
            kb.op("pe", lambda e, ps=ps, kc=kc, tt=tt: e.matmul(ps[:, 0:32], lhsT=xT[:, kc, tt * 128:(tt + 1) * 128], rhs=wr[:, kc, :],
                                                                start=(kc == 0), stop=(kc == 15)), [wr, xT], [ps])
        kb.op("dve", lambda e, ps=ps: e.tensor_tensor(out=lg[:], in0=ps[:, 0:32], in1=brt[:], op=ALU.add), [ps, brt], [lg])
        kb.op("dve", lambda e: e.max(out=m8[:], in_=lg[:]), [lg], [m8])
        kb.op("dve", lambda e: e.tensor_scalar(out=msk[:], in0=lg[:], scalar1=m8[:, 3:4], scalar2=None, op0=ALU.is_ge), [lg, m8], [msk])
        kb.op("dve", lambda e: e.tensor_scalar_mul(out=st[:, 0:1], in0=m8[:, 0:1], scalar1=-1.0), [m8], [st])
        kb.op("act", lambda e: e.activation(out=ex[:], in_=lg[:], func=AF.Exp, bias=st[:, 0:1]), [lg, st], [ex])
        kb.op("dve", lambda e: e.tensor_tensor(out=ex[:], in0=ex[:], in1=msk[:], op=ALU.mult), [ex, msk], [ex])
        kb.op("dve", lambda e: e.reduce_sum(out=st[:, 1:2], in_=ex[:], axis=AX.X), [ex, st], [st])
        kb.op("dve", lambda e: e.reciprocal(out=st[:, 2:3], in_=st[:, 1:2]), [st], [st])
        kb.op("dve", lambda e, tt=tt: e.tensor_scalar_mul(out=rw[:, tt, :], in0=ex[:], scalar1=st[:, 2:3]), [ex, st], [rw])
        kb.op("dve", lambda e, tt=tt: e.tensor_copy(out=rwb[:], in_=rw[:, tt, :]), [rw], [rwb])
        kb.op("pe", lambda e: e.transpose(out=mk.PT[0:32, 0:128], in_=rwb[:], identity=mk.c["ident"][:]), [rwb, mk.c["ident"]], [mk.PT])
        mk.evac(rwT[:, tt, :], mk.PT[0:32, 0:128], [mk.PT], [rwT])
    bgc = kb.sb("bgc", [128, 128], F32)
    buc = kb.sb("buc", [128, 128], F32)
    tb = kb.sb("tbias", [128, 128], F32)
    tbb = kb.sb("tbiasb", [128, 128], BF16)
    for nm, dst in (("moe_b_gate", bgc), ("moe_b_up", buc)):
        kb.dma("sp", tb[:], mk.v_in[nm][l:l + 1, :].rearrange("o (q p) -> (o q) p", p=128), reads=[mk.v_in[nm]], writes=[tb])
        kb.op("dve", lambda e: e.tensor_copy(out=tbb[:], in_=tb[:]), [tb], [tbb])
        kb.op("pe", lambda e: e.transpose(out=mk.PT[:, 0:128], in_=tbb[:], identity=mk.c["ident"][:]), [tbb, mk.c["ident"]], [mk.PT])
        mk.evac(dst[:], mk.PT[:, 0:128], [mk.PT], [dst])
    bdb = kb.sb("bdb", [32, D], BF16)
    mB = kb.mark()
    bdf = kb.sb("bdf", [32, D], F32)
    kb.dma("sp", bdf[:], mk.v_in["moe_b_down"][l:l + 1, :].rearrange("o (e n) -> (o e) n", n=D), reads=[mk.v_in["moe_b_down"]], writes=[bdf])
    kb.op("dve", lambda e: e.tensor_copy(out=bdb[:], in_=bdf[:]), [bdf], [bdb])
    kb.release(mB)
    acc = [[kb.sb("accG%d_%d" % (t_, n_), [128, 512], F32) for n_ in range(4)] for t_ in range(4)]
    wg = kb.sb("wgG", [128, 16, 512], BF16)
    wu = kb.sb("wuG", [128, 16, 512], BF16)
    wd = kb.sb("wdG", [128, 4, D], BF16)
    hT = kb.sb("hTG", [128, 4, 512], BF16)
    gs = kb.sb("gG", [128, 512], F32)
    sg = kb.sb("sgG", [128, 512], F32)
    us = kb.sb("uG", [128, 512], F32)
    xt = kb.sb("xtG", [128, D], F32)
    wgs = Wg.ap().rearrange("(e kc p) n -> e p kc n", e=32, p=128)
    wus = Wu.ap().rearrange("(e kc p) n -> e p kc n", e=32, p=128)
    wds = Wd.ap().rearrange("(e kc p) n -> e p kc n", e=32, p=128)
    for i in range(NQC):
        for tt in range(4):
            for n4 in range(4):
                ps = mk.nextP()
                kb.op("pe", lambda e, ps=ps, tt=tt, n4=n4, i=i: e.matmul(ps[:], lhsT=rwT[:, i * 4 + tt, :], rhs=bdb[:, n4 * 512:(n4 + 1) * 512],
                                                                        start=True, stop=True), [rwT, bdb], [ps])
                mk.evac(acc[tt][n4][:], ps[:], [ps], [acc[tt][n4]])
        for ex_ in range(32):
            kb.dma("sp", wg[:], wgs[ex_], reads=[Wg], writes=[wg])
            kb.dma("sp", wu[:], wus[ex_], reads=[Wu], writes=[wu])
            kb.dma("sp", wd[:], wds[ex_], reads=[Wd], writes=[wd])
            for fc in range(4):
                pg = mk.nextP()
                for kc in range(16):
                    kb.op("pe", lambda e, pg=pg, kc=kc, fc=fc, i=i: e.matmul(pg[:], lhsT=wg[:, kc, fc * 128:(fc + 1) * 128], rhs=xT[:, kc, i * 512:(i + 1) * 512],
                                                                            start=(kc == 0), stop=(kc == 15)), [wg, xT], [pg])
                pu = mk.nextP()
                for kc in range(16):
                    kb.op("pe", lambda e, pu=pu, kc=kc, fc=fc, i=i: e.matmul(pu[:], lhsT=wu[:, kc, fc * 128:(fc + 1) * 128], rhs=xT[:, kc, i * 512:(i + 1) * 512],
                                                                            start=(kc == 0), stop=(kc == 15)), [wu, xT], [pu])
                col = ex_ * 4 + fc
                kb.op("dve", lambda e, pg=pg, col=col: e.tensor_scalar(out=gs[:], in0=pg[:], scalar1=bgc[:, col:col + 1], scalar2=7.0,
                                                                      op0=ALU.add, op1=ALU.min), [pg, bgc], [gs])
                kb.op("act", lambda e: e.activation(out=sg[:], in_=gs[:], func=AF.Sigmoid, scale=1.702), [gs], [sg])
                kb.op("dve", lambda e, pu=pu, col=col: e.tensor_scalar(out=us[:], in0=pu[:], scalar1=buc[:, col:col + 1], scalar2=7.0,
                                                                      op0=ALU.add, op1=ALU.min), [pu, buc], [us])
                kb.op("dve", lambda e: e.tensor_scalar(out=us[:], in0=us[:], scalar1=-7.0, scalar2=1.0, op0=ALU.max, op1=ALU.add), [us], [us])
                kb.op("dve", lambda e: e.tensor_tensor(out=gs[:], in0=gs[:], in1=sg[:], op=ALU.mult), [gs, sg], [gs])
                kb.op("dve", lambda e, fc=fc: e.tensor_tensor(out=hT[:, fc, :], in0=gs[:], in1=us[:], op=ALU.mult), [gs, us], [hT])
            for tt in range(4):
                for n4 in range(4):
                    ps = mk.PO[n4]
                    a_ = acc[tt][n4]
                    for fc in range(4):
                        kb.op("pe", lambda e, ps=ps, fc=fc, tt=tt, n4=n4: e.matmul(ps[:], lhsT=hT[:, fc, tt * 128:(tt + 1) * 128],
                                                                                 rhs=wd[:, fc, n4 * 512:(n4 + 1) * 512], start=(fc == 0), stop=(fc == 3)),
                              [hT, wd], [ps])
                    kb.op("dve", lambda e, ps=ps, tt=tt, a_=a_, i=i, ex_=ex_: e.scalar_tensor_tensor(
                        out=a_[:], in0=ps[:], scalar=rw[:, i * 4 + tt, ex_:ex_ + 1],
                        in1=a_[:], op0=ALU.mult, op1=ALU.add), [ps, rw, a_], [a_])
        for tt in range(4):
            r0 = i * 512 + tt * 128
            kb.dma("sp", xt[:], xsrc[r0:r0 + 128, :], reads=[xsrc], writes=[xt])
            for n4 in range(4):
                kb.op("dve", lambda e, tt=tt, n4=n4: e.scalar_tensor_tensor(out=xt[:, n4 * 512:(n4 + 1) * 512], in0=xt[:, n4 * 512:(n4 + 1) * 512],
                                                                          scalar=DN_ALPHA, in1=acc[tt][n4][:], op0=ALU.mult, op1=ALU.add),
                      [xt, acc[tt][n4]], [xt])
            ln_tile(mk, xt, gB, bB, xt, junk=hT)
            kb.dma("sp", xdst[r0:r0 + 128, :], xt[:], reads=[xt], writes=[xdst])
    mk.end_stage(m0)


def build_layers(mk, L, layer_ids=None):
    kb = mk.kb
    mk.consts()
    mk.gather_weights()
    xT = kb.sb("xT", [128, 16, T], BF16)
    gates = kb.sb("gates", [128, 16, 24], F32)
    xcur = mk.x_in
    for l in range(L):
        last = (l == L - 1)
        mk.make_xT(xcur, xT, l, "x")
        xtg = mk.exchange_xT(xT, l)
        stage_A(mk, l, xtg)
        stage_B_mla(mk, l, xT)
        stage_B_rest(mk, l, xT, gates)
        OT = kb.dram("OT_%d" % l, [3 * 1024, T], BF16)
        mla_attention(mk, l, OT)
        nsa_attention(mk, l, OT, gates)
        sb_attention(mk, l, OT)
        mk.OT = OT
        x1 = kb.dram("x1_%d" % l, [T, D], F32)
        stage_E(mk, l, xT, OT, xcur, x1)
        mk.make_xT(x1, xT, l, "x1")
        x2 = kb.dram("x2_%d" % l, [T, D], F32)
        stage_F(mk, l, xT, x1, x2)
        mk.make_xT(x2, xT, l, "x2")
        x3 = mk.out if last else kb.dram("x3_%d" % l, [T, D], F32)
        stage_G(mk, l, xT, x2, x3)
        mk.xs = (x1, x2, x3)
        xcur = x3


def make_tables(hf):
    t = {}
    t["t_ident"] = np.eye(128, dtype=np.float32)
    tri = np.zeros((128, 256), np.float32)
    j = np.arange(128)
    tri[:, :128] = (j[:, None] >= j[None, :])
    tri[:, 128:] = 1.0
    t["t_tri"] = tri
    inv = (10000.0 ** (-np.arange(0, 64, 2) / 64)).astype(np.float32)
    pos = np.arange(S, dtype=np.float32)
    ang = pos[:, None] * inv[None, :]
    rope_all = np.concatenate([np.cos(ang), np.sin(ang)], 1).astype(np.float32)
    own_pos = np.concatenate([np.arange(512) + 512 * c for c in OWN[hf]])
    t["t_rope_all"] = rope_all
    t["t_rope_own"] = rope_all[own_pos]
    sr = np.arange(128)
    tr = np.arange(512)
    mI = np.zeros((NQC, 8, 128, 512), np.float32); mS = np.zeros_like(mI)
    mW = np.zeros((NQC, 12, 128, 512), np.float32)
    mC = np.zeros((NQC, 2, 128, 512), np.float32)
    bcol = np.zeros((128, NQC, 32, 8), np.float32)
    bcolc = np.zeros((128, NQC, 2, 8), np.float32)
    for i in range(NQC):
        J = OWN[hf][i]
        tpos = 512 * J + tr
        nkt = 4 * (JMAX[i] + 1)
        for r in range(8):
            spos = 128 * (nkt - 8 + r) + sr
            mI[i, r] = np.where(spos[:, None] <= tpos[None, :], 0.0, NEGM)
            mS[i, r] = np.where(spos[:, None] < tpos[None, :], 0.0, NEGM)
        for r in range(12):
            spos = 128 * (nkt - 12 + r) + sr
            dw = tpos[None, :] - spos[:, None]
            mW[i, r] = np.where((dw >= 0) & (dw < 512) & (spos[:, None] >= 0), 0.0, NEGM)
        for nq in range(2):
            n = 128 * nq + sr
            ok = (16 * n[:, None] + 31 <= tpos[None, :]) & (n[:, None] < 255)
            mC[i, nq] = np.where(ok, 0.0, NEGM)
        for kt in range(32):
            for h in range(8):
                bcol[:, i, kt, h] = -SLOPES[h] * (512 * J - 128 * kt)
        for nq in range(2):
            for h in range(8):
                bcolc[:, i, nq, h] = -SLOPES[h] * (512 * J - 2048 * nq)
    t["t_maskI"] = mI.reshape(-1, 512); t["t_maskS"] = mS.reshape(-1, 512)
    t["t_maskW"] = mW.reshape(-1, 512); t["t_maskC"] = mC.reshape(-1, 512)
    t["t_bcol"] = bcol.reshape(128, -1); t["t_bcolc"] = bcolc.reshape(128, -1)
    alk = np.zeros((4, 8, 128), np.float32); alq = np.zeros((4, 8, 512), np.float32); alck = np.zeros((4, 8, 128), np.float32)
    for h in range(8):
        sl = SLOPES[h]
        alk[0, h] = 1.0; alk[1, h] = 1.0; alk[2, h] = sl * sr
        alq[0, h] = -sl * (tr % 128); alq[1, h] = -sl * 128 * (tr // 128); alq[2, h] = 1.0; alq[3, h] = 1.0
        alck[0, h] = 1.0; alck[1, h] = 1.0; alck[2, h] = sl * 16 * sr; alck[3, h] = sl * 15.5
    t["t_alk"] = alk.reshape(4, -1); t["t_alq"] = alq.reshape(4, -1); t["t_alck"] = alck.reshape(4, -1)
    own_pos_t = own_pos
    blk = np.arange(64)
    cur = own_pos_t // 64
    forced = (blk[None, :] == 0) | (blk[None, :] == cur[:, None]) | (blk[None, :] == cur[:, None] - 1)
    fb = np.where(blk[None, :] > cur[:, None], -100.0, np.where(forced, 100.0, 0.0)).astype(np.float32)
    t["t_fb"] = fb
    cs_ = np.arange(255) * 16; ss_ = np.arange(64) * 64
    ov = np.clip(np.minimum(cs_[:, None] + 32, ss_[None, :] + 64) - np.maximum(cs_[:, None], ss_[None, :]), 0, None) / 32
    ovp = np.zeros((256, 64), np.float32); ovp[:255] = ov
    t["t_ov"] = ovp
    et = np.zeros((64, S), np.float32)
    et[np.arange(S) // 64, np.arange(S)] = 1.0
    t["t_et"] = et
    return t


def make_in_maps(inputs, L):
    maps = []
    tabs = [make_tables(0), make_tables(1)]
    for c in range(8):
        b, hf = c // 2, c % 2
        own_pos = np.concatenate([np.arange(512) + 512 * cc for cc in OWN[hf]])
        m = {"x_own": np.ascontiguousarray(inputs["x"][b][own_pos]), "mem_b": np.ascontiguousarray(inputs["mem"][b])}
        for n, (r, cdim) in WSPEC.items():
            w = inputs[n][:L].reshape(L, r, cdim)
            m["ws_" + n] = np.ascontiguousarray(w[:, c * (r // 8):(c + 1) * (r // 8), :]).reshape(L * (r // 8), cdim)
        for n, ln in VSPEC.items():
            m["vs_" + n] = np.ascontiguousarray(inputs[n][:L].reshape(L, ln))
        m.update(tabs[hf])
        maps.append(m)
    return maps


_PROG = {}


def _program(L):
    if L not in _PROG:
        mk = MK(L)
        build_layers(mk, L)
        mk.kb.wait_all("sp", [mk.out])
        mk.kb.barrier()
        _PROG[L] = mk.kb.finish()
    return _PROG[L]


def _gather_out(res):
    out = np.zeros((4, S, D), np.float32)
    for c in range(8):
        b, hf = c // 2, c % 2
        own_pos = np.concatenate([np.arange(512) + 512 * cc for cc in OWN[hf]])
        out[b][own_pos] = np.asarray(res.results[c]["y_out"], dtype=np.float32)
    return out


def kernel(**inputs):
    from concourse.bass_utils import run_bass_kernel_spmd
    inputs = {k: np.asarray(v) for k, v in inputs.items()}
    if FUSED:
        nc = _program(4)
        res = run_bass_kernel_spmd(nc, make_in_maps(inputs, 4), core_ids=list(range(8)))
        return _gather_out(res)
    nc = _program(1)
    x = inputs["x"]
    for l in range(4):
        lay = {k: (v[l:l + 1] if k not in ("x", "mem") else v) for k, v in inputs.items()}
        lay["x"] = x
        res = run_bass_kernel_spmd(nc, make_in_maps(lay, 1), core_ids=list(range(8)))
        x = _gather_out(res)
    return x
```
